# Optimizing a Trainium2 kernel written in Bass

```python
import jax, jax.numpy as jnp
from jax import lax
import numpy as np

D_MODEL = 1024
BATCH = 4
SEQ = 4096
DEPTH = 4

PLE_DIM = 256
GLA_HEADS = 4
GLA_DK = D_MODEL // 8
GLA_DV = D_MODEL // 4
GLA_GATE_RANK = 16
GLA_GATE_TAU = 16.0
GLA_CHUNK = 64
MOBA_HEADS = 8
MOBA_DH = D_MODEL // 8
MOBA_BLOCK = 256
MOBA_TOPK = 3
MOBA_QCHUNK = 32
D_FF = 4 * D_MODEL
EPS = 1e-6

IN_SPLITS = (
    GLA_HEADS * GLA_DK,
    GLA_HEADS * GLA_DK,
    GLA_HEADS * GLA_DV,
    GLA_GATE_RANK,
    GLA_HEADS * GLA_DV,
    MOBA_HEADS * MOBA_DH,
    MOBA_HEADS * MOBA_DH,
    MOBA_HEADS * MOBA_DH,
    D_MODEL,
    D_MODEL,
)
N_IN = sum(IN_SPLITS)

kernel_name = 'hybrid_gla_moba_gated_block'


def rms_norm(x, g):
    xf = x.astype(jnp.float32)
    y = xf * lax.rsqrt(jnp.mean(xf * xf, axis=-1, keepdims=True) + EPS)
    return (y * g.astype(jnp.float32)).astype(x.dtype)


def gla_chunked(q, k, v, log_a):
    B, S, H, dk = q.shape
    dv = v.shape[-1]
    n = S // GLA_CHUNK

    def chunk(t):
        return t.astype(jnp.float32).reshape(B, n, GLA_CHUNK, H, t.shape[-1]).transpose(0, 3, 1, 2, 4)

    q, k, v, log_a = chunk(q), chunk(k), chunk(v), chunk(log_a)
    b = jnp.cumsum(log_a, axis=3)
    b_last = b[:, :, :, -1:, :]
    qd = q * jnp.exp(b) * (dk ** -0.5)
    kd = k * jnp.exp(-b)
    kl = k * jnp.exp(b_last - b)
    kv = jnp.einsum('bhncd,bhnce->bhnde', kl, v)
    decay = jnp.exp(b_last[:, :, :, 0, :])

    def step(state, inp):
        dec, kv_n = inp
        return dec[..., None] * state + kv_n, state

    s0 = jnp.zeros((B, H, dk, dv), jnp.float32)
    _, s_prev = lax.scan(step, s0, (jnp.moveaxis(decay, 2, 0), jnp.moveaxis(kv, 2, 0)))
    s_prev = jnp.moveaxis(s_prev, 0, 2)
    o_inter = jnp.einsum('bhncd,bhnde->bhnce', qd, s_prev)
    causal = jnp.tril(jnp.ones((GLA_CHUNK, GLA_CHUNK), bool))
    a = jnp.where(causal, jnp.einsum('bhncd,bhnsd->bhncs', qd, kd), 0.0)
    o = o_inter + jnp.einsum('bhncs,bhnse->bhnce', a, v)
    return o.transpose(0, 2, 3, 1, 4).reshape(B, S, H, dv)


def moba_attention(q, k, v):
    B, S, H, dh = q.shape
    s_pad = -(-S // MOBA_BLOCK) * MOBA_BLOCK
    nb = s_pad // MOBA_BLOCK
    n_sel = min(MOBA_TOPK, max(nb - 1, 1))
    nq = s_pad // MOBA_QCHUNK
    pad = ((0, 0), (0, s_pad - S), (0, 0), (0, 0))
    q, k, v = [jnp.pad(t, pad).transpose(0, 2, 1, 3) for t in (q, k, v)]
    kb = k.reshape(B, H, nb, MOBA_BLOCK, dh)
    vb = v.reshape(B, H, nb, MOBA_BLOCK, dh)
    scale = dh ** -0.5
    slopes = 2.0 ** (-8.0 * jnp.arange(1, H + 1, dtype=jnp.float32) / H)

    k_mean = jnp.mean(kb.astype(jnp.float32), axis=3)
    gate = jnp.einsum('bhsd,bhnd->bhsn', q.astype(jnp.float32), k_mean)
    pos = jnp.arange(s_pad)
    fully_past = jnp.arange(nb)[None, :] < (pos // MOBA_BLOCK)[:, None]
    gate = jnp.where(fully_past, gate, -jnp.inf)
    top_val, top_idx = lax.top_k(gate, n_sel)
    valid = jnp.isfinite(top_val)

    def to_chunks(t):
        return jnp.moveaxis(t.reshape(B, H, nq, MOBA_QCHUNK, *t.shape[3:]), 2, 0)

    b_ix = jnp.arange(B)[:, None, None, None]
    h_ix = jnp.arange(H)[None, :, None, None]

    def attend(args):
        q_c, idx_c, valid_c, c = args
        t = c * MOBA_QCHUNK + jnp.arange(MOBA_QCHUNK)
        blk = (c * MOBA_QCHUNK) // MOBA_BLOCK
        k_own = lax.dynamic_index_in_dim(kb, blk, axis=2, keepdims=False)
        v_own = lax.dynamic_index_in_dim(vb, blk, axis=2, keepdims=False)
        own_pos = blk * MOBA_BLOCK + jnp.arange(MOBA_BLOCK)
        d_own = (t[:, None] - own_pos[None, :]).astype(jnp.float32)
        sc_own = (jnp.einsum('bhqd,bhkd->bhqk', q_c, k_own).astype(jnp.float32) * scale
                  - slopes[:, None, None] * jnp.abs(d_own))
        sc_own = jnp.where(d_own >= 0, sc_own, -jnp.inf)
        k_sel = kb[b_ix, h_ix, idx_c]
        v_sel = vb[b_ix, h_ix, idx_c]
        sel_pos = idx_c[..., None] * MOBA_BLOCK + jnp.arange(MOBA_BLOCK)
        d_sel = (t[:, None, None] - sel_pos).astype(jnp.float32)
        sc_sel = (jnp.einsum('bhqd,bhqnkd->bhqnk', q_c, k_sel).astype(jnp.float32) * scale
                  - slopes[:, None, None, None] * jnp.abs(d_sel))
        sc_sel = jnp.where(valid_c[..., None], sc_sel, -jnp.inf)
        scores = jnp.concatenate(
            [sc_own, sc_sel.reshape(B, H, MOBA_QCHUNK, n_sel * MOBA_BLOCK)], axis=-1)
        prob = jax.nn.softmax(scores, axis=-1).astype(v.dtype)
        p_own = prob[..., :MOBA_BLOCK]
        p_sel = prob[..., MOBA_BLOCK:].reshape(B, H, MOBA_QCHUNK, n_sel, MOBA_BLOCK)
        return (jnp.einsum('bhqk,bhkd->bhqd', p_own, v_own)
                + jnp.einsum('bhqnk,bhqnkd->bhqd', p_sel, v_sel))

    out = lax.map(attend, (to_chunks(q), to_chunks(top_idx), to_chunks(valid), jnp.arange(nq)))
    out = jnp.moveaxis(out, 0, 2).reshape(B, H, s_pad, dh)[:, :, :S]
    return out.transpose(0, 2, 1, 3).reshape(B, S, H * dh)


def setup_inputs(seed: int = 0) -> dict:
    key = jax.random.key(seed)
    ks = jax.random.split(key, 20)

    def nrm(k, shape, fan_in, scale=1.0):
        return jax.random.normal(k, shape, jnp.float32) * (scale * fan_in ** -0.5)

    def gain(k, shape):
        return 1.0 + 0.05 * jax.random.normal(k, shape, jnp.float32)

    return {
        'x': jax.random.normal(ks[0], (BATCH, SEQ, D_MODEL), jnp.float32),
        'p': jax.random.normal(ks[1], (DEPTH, BATCH, SEQ, PLE_DIM), jnp.float32),
        'norm_mix': gain(ks[2], (DEPTH, D_MODEL)),
        'w_in': nrm(ks[3], (DEPTH, D_MODEL, N_IN), D_MODEL),
        'gla_gate_w2': nrm(ks[4], (DEPTH, GLA_GATE_RANK, GLA_HEADS * GLA_DK), GLA_GATE_RANK),
        'gla_gate_b': 0.1 * jax.random.normal(ks[5], (DEPTH, GLA_HEADS * GLA_DK), jnp.float32),
        'gla_out_norm': gain(ks[6], (DEPTH, GLA_HEADS, GLA_DV)),
        'moba_q_norm': gain(ks[7], (DEPTH, MOBA_DH)),
        'moba_k_norm': gain(ks[8], (DEPTH, MOBA_DH)),
        'w_branch_a': nrm(ks[9], (DEPTH, GLA_HEADS * GLA_DV, D_MODEL), GLA_HEADS * GLA_DV),
        'w_branch_b': nrm(ks[10], (DEPTH, MOBA_HEADS * MOBA_DH, D_MODEL), MOBA_HEADS * MOBA_DH),
        'w_out': nrm(ks[11], (DEPTH, D_MODEL, D_MODEL), D_MODEL, 0.5),
        'norm_mlp': gain(ks[12], (DEPTH, D_MODEL)),
        'w_up': nrm(ks[13], (DEPTH, D_MODEL, D_FF), D_MODEL),
        'w_down': nrm(ks[14], (DEPTH, D_FF, D_MODEL), D_FF, 0.5),
        'norm_ple': gain(ks[15], (DEPTH, D_MODEL)),
        'w_ple_gate': nrm(ks[16], (DEPTH, D_MODEL, D_MODEL), D_MODEL),
        'w_ple': nrm(ks[17], (DEPTH, PLE_DIM, D_MODEL), PLE_DIM, 0.5),
    }


def reference(x, p, norm_mix, w_in, gla_gate_w2, gla_gate_b, gla_out_norm, moba_q_norm,
              moba_k_norm, w_branch_a, w_branch_b, w_out, norm_mlp, w_up, w_down,
              norm_ple, w_ple_gate, w_ple):
    B, S, _ = x.shape
    points = []
    acc = 0
    for sz in IN_SPLITS[:-1]:
        acc += sz
        points.append(acc)

    for i in range(DEPTH):
        h = rms_norm(x, norm_mix[i])
        u = h @ w_in[i]
        (gq, gk, gv, g_lr, g_r, mq, mk, mv, gate_a, gate_b) = jnp.split(u, points, axis=-1)

        z = (g_lr @ gla_gate_w2[i] + gla_gate_b[i]).astype(jnp.float32)
        log_a = jax.nn.log_sigmoid(z) / GLA_GATE_TAU
        o_a = gla_chunked(gq.reshape(B, S, GLA_HEADS, GLA_DK),
                          gk.reshape(B, S, GLA_HEADS, GLA_DK),
                          gv.reshape(B, S, GLA_HEADS, GLA_DV),
                          log_a.reshape(B, S, GLA_HEADS, GLA_DK)).astype(x.dtype)
        o_a = rms_norm(o_a, gla_out_norm[i]).reshape(B, S, GLA_HEADS * GLA_DV) * jax.nn.silu(g_r)
        y_a = o_a @ w_branch_a[i]

        qh = rms_norm(mq.reshape(B, S, MOBA_HEADS, MOBA_DH), moba_q_norm[i])
        kh = rms_norm(mk.reshape(B, S, MOBA_HEADS, MOBA_DH), moba_k_norm[i])
        vh = mv.reshape(B, S, MOBA_HEADS, MOBA_DH)
        y_b = moba_attention(qh, kh, vh) @ w_branch_b[i]

        y = jax.nn.sigmoid(gate_a) * y_a + jax.nn.sigmoid(gate_b) * y_b
        x = x + y @ w_out[i]

        h2 = rms_norm(x, norm_mlp[i])
        x = x + jnp.square(jax.nn.relu(h2 @ w_up[i])) @ w_down[i]

        ple_gate = jax.nn.sigmoid(rms_norm(x, norm_ple[i]) @ w_ple_gate[i])
        x = x + ple_gate * (p[i] @ w_ple[i])
    return x
```

```python
import contextlib
import numpy as np
import concourse.bass as bass
import concourse.mybir as mybir
from concourse.bass_utils import run_bass_kernel_spmd

F32 = mybir.dt.float32
BF16 = mybir.dt.bfloat16
ALU = mybir.AluOpType
AF = mybir.ActivationFunctionType

ENGS = ("pe", "act", "dve", "pool", "sp")


class Buf:
    __slots__ = ("name", "lw", "rd", "excl", "ver")

    def __init__(self, name, excl=False):
        self.name = name
        self.lw = None
        self.rd = []
        self.ver = 0
        self.excl = excl


class PV:
    __slots__ = ("buf", "ver")

    def __init__(self, buf):
        buf.ver += 1
        self.buf = buf
        self.ver = buf.ver


def _res(bufs):
    out = []
    for b in bufs:
        if isinstance(b, PV):
            assert b.ver == b.buf.ver, "stale PSUM bank handle %s" % b.buf.name
            b = b.buf
        out.append(b)
    return out


class Key:
    __slots__ = ("idx", "cnt", "ldma")

    def __init__(self, idx):
        self.idx = idx
        self.cnt = 0
        self.ldma = None


class Op:
    __slots__ = ("eng", "fn", "deps", "sig", "sigval", "dma", "key", "semval")

    def __init__(self, eng, fn):
        self.eng = eng
        self.fn = fn
        self.deps = []
        self.sig = False
        self.sigval = 0
        self.dma = False
        self.key = None
        self.semval = 0


class Sched:
    def __init__(self, nc):
        self.nc = nc
        self.ops = {e: [] for e in ENGS}
        self.keys = {}

    def key(self, name):
        k = self.keys.get(name)
        if k is None:
            k = Key(len(self.keys))
            self.keys[name] = k
        return k

    def _track(self, op, reads, writes):
        reads = _res(reads)
        writes = _res(writes)
        deps = {}
        for b in reads:
            if b.lw is not None:
                deps[id(b.lw)] = b.lw
            if b.excl:
                for r in b.rd:
                    if r.eng != op.eng:
                        deps[id(r)] = r
        for b in writes:
            if b.lw is not None:
                deps[id(b.lw)] = b.lw
            for r in b.rd:
                deps[id(r)] = r
        for d in deps.values():
            if d is op:
                continue
            if (not d.dma) and d.eng == "pe" and op.eng == "pe" and not op.dma:
                continue
            op.deps.append(d)
            if not d.dma:
                d.sig = True
        for b in reads:
            b.rd.append(op)
        for b in writes:
            b.lw = op
            b.rd = []

    def op(self, eng, fn, reads=(), writes=()):
        o = Op(eng, fn)
        self._track(o, reads, writes)
        self.ops[eng].append(o)
        return o

    def dma(self, q, out_ap, in_ap, reads=(), writes=(), key=None):
        key = self.key(key)
        o = Op(q, lambda e: e.dma_start(out=out_ap, in_=in_ap))
        o.dma = True
        key.cnt += 16
        o.key = key
        o.semval = key.cnt
        self._track(o, reads, writes)
        if key.ldma is not None and all(d is not key.ldma for d in o.deps):
            o.deps.append(key.ldma)
        key.ldma = o
        self.ops[q].append(o)
        return o

    def cc(self, kind, groups, in_ap, out_ap, reads=(), writes=(), key=None):
        key = self.key(key)
        o = Op("pool", lambda e: e.collective_compute(kind, op=ALU.bypass, replica_groups=groups, ins=[in_ap], outs=[out_ap]))
        o.dma = True
        key.cnt += 16
        o.key = key
        o.semval = key.cnt
        self._track(o, reads, writes)
        if key.ldma is not None and all(d is not key.ldma for d in o.deps):
            o.deps.append(key.ldma)
        key.ldma = o
        self.ops["pool"].append(o)
        return o

    def barrier(self):
        lasts = []
        for e in ("pe", "act", "dve", "pool"):
            for o in reversed(self.ops[e]):
                if not o.dma and o.fn is not None:
                    lasts.append(o)
                    break
        dmas = [k.ldma for k in self.keys.values() if k.ldma is not None]
        for e in ENGS:
            o = Op(e, None)
            for d in lasts + dmas:
                if d.eng == "pe" and e == "pe" and not d.dma:
                    continue
                o.deps.append(d)
                if not d.dma:
                    d.sig = True
            self.ops[e].append(o)

    def emit(self):
        nc = self.nc
        with contextlib.ExitStack() as st:
            esem = {e: st.enter_context(nc.semaphore("s_" + e)) for e in ("pe", "act", "dve", "pool")}
            dsem = [st.enter_context(nc.semaphore("d%d" % i)) for i in range(len(self.keys))]
            for e in ("pe", "act", "dve", "pool"):
                c = 0
                for o in self.ops[e]:
                    if o.sig and not o.dma:
                        c += 1
                        o.sigval = c
            block = st.enter_context(nc.Block())

            def run(e, eng):
                waited = {}
                for o in self.ops[e]:
                    for d in o.deps:
                        if d.dma:
                            sem, val = dsem[d.key.idx], d.semval
                        else:
                            sem, val = esem[d.eng], d.sigval
                        k = id(sem)
                        if waited.get(k, 0) < val:
                            eng.wait_ge(sem, val)
                            waited[k] = val
                    if o.fn is None:
                        continue
                    ins = o.fn(eng)
                    if o.dma:
                        ins.then_inc(dsem[o.key.idx], 16)
                    elif o.sig:
                        ins.then_inc(esem[e], 1)

            @block.tensor
            def _(eng):
                run("pe", eng)

            @block.scalar
            def _(eng):
                run("act", eng)

            @block.vector
            def _(eng):
                run("dve", eng)

            @block.gpsimd
            def _(eng):
                run("pool", eng)

            @block.sync
            def _(eng):
                run("sp", eng)


D = 1024
KC = 8
NT = 4096
TT = 512
NTT = 8
DEPTH = 4
PLE = 256
DFF = 4096
O_GQ, O_GK, O_GV, O_GLR, O_GR, O_MQ, O_MK, O_MV, O_GA, O_GB, N_IN = (
    0, 512, 1024, 2048, 2064, 3088, 4112, 5136, 6160, 7184, 8208)
EPS = 1e-6
NV_L = 34
NEG = -30000.0
CF_ID, CF_TRI, CF_TRI2, CF_MBD, CF_GM, CF_AB, NCF = 0, 128, 256, 384, 512, 1024, 1536
CB_ONE, CB_ID, CB_CM, NCB = 0, 128, 256, 512


def host_consts():
    cf = np.zeros((128, NCF), np.float32)
    cf[:, CF_ID:CF_ID + 128] = np.eye(128, dtype=np.float32)
    s = np.arange(128)[:, None]
    t = np.arange(128)[None, :]
    same = (s // 64) == (t // 64)
    cf[:, CF_TRI:CF_TRI + 128] = np.where(same & (s <= t), -1.0 / 16.0, 0.0)
    cf[:, CF_TRI2:CF_TRI2 + 128] = np.where(same & (s > t), -1.0 / 16.0, 0.0)
    cf[:, CF_MBD:CF_MBD + 128] = np.where(same & (s <= t), 1.0, 0.0)
    b = (np.arange(32) // 2)[:, None]
    j = np.arange(16)[None, :]
    gm = np.where(j < b, 0.0, np.where(j == b, 1e30, -1e30)).astype(np.float32)
    cf[:, CF_GM:CF_GM + 512] = gm.reshape(1, 512)
    ab = (-256.0 * (b - j)).astype(np.float32)
    cf[:, CF_AB:CF_AB + 512] = ab.reshape(1, 512)
    cb = np.zeros((128, NCB), np.float32)
    cb[:, CB_ONE:CB_ONE + 128] = 1.0
    cb[:, CB_ID:CB_ID + 128] = np.eye(128, dtype=np.float32)
    kk = np.arange(128)[:, None]
    qq = np.arange(256)[None, :]
    cb[:, CB_CM:CB_CM + 256] = np.where(kk > qq, NEG, 0.0)
    laug = np.zeros((128, 32, 128), np.float32)
    for jj in range(16):
        for c in range(2):
            laug[jj, jj * 2 + c, :] = 1.0
            laug[16, jj * 2 + c, :] = c * 128 + np.arange(128)
            laug[17, jj * 2 + c, :] = 1.0
    laug = laug.reshape(128, 4096)
    slopes = 2.0 ** (-(np.arange(8) + 1.0))
    augc = np.zeros((8, 2, NT), np.float32)
    trel = (np.arange(NT) % 256).astype(np.float32)
    for h in range(8):
        augc[h, 0, :] = slopes[h]
        augc[h, 1, :] = -slopes[h] * trel
    return cf, cb, laug, augc, slopes


def build(depth=DEPTH, debug=False, stop_after=None, dbg=None):
    nc = bass.Bass("TRN2", target_bir_lowering=False)
    slopes = [2.0 ** (-(h + 1.0)) for h in range(8)]

    def din(name, shape, dt=F32):
        return nc.dram_tensor(name, shape, dt, kind="ExternalInput").ap()

    xT_d = din("xT", [D, NT])
    pT_d = din("pT", [DEPTH, PLE, NT])
    w_in_d = din("w_in", [DEPTH, D, N_IN])
    wa_d = din("w_branch_a", [DEPTH, D, D])
    wb_d = din("w_branch_b", [DEPTH, D, D])
    wo_d = din("w_out", [DEPTH, D, D])
    wup_d = din("w_up", [DEPTH, D, DFF])
    wdn_d = din("w_down", [DEPTH, DFF, D])
    wpg_d = din("w_ple_gate", [DEPTH, D, D])
    wple_d = din("w_ple", [DEPTH, PLE, D])
    w2aug_d = din("w2aug", [DEPTH, 128, 512])
    cf_d = din("cf32", [128, NCF])
    cb_d = din("cbf", [128, NCB])
    laug_d = din("laug", [128, 4096])
    augc_d = din("augc", [8, 2, NT])
    vecs_d = din("vecs", [128, DEPTH * NV_L])
    out_d = nc.dram_tensor("outT", [D, NT], F32, kind="ExternalOutput").ap()
    dk = "ExternalOutput" if debug else "Internal"
    xS_d = nc.dram_tensor("xS", [D, NT], F32, kind=dk).ap()
    oaT_d = nc.dram_tensor("oaT", [8, 128, NT], BF16, kind=dk).ap()
    obT_d = nc.dram_tensor("obT", [8, 128, NT], BF16, kind=dk).ap()

    def kcp(ap2d):
        return ap2d.rearrange("(kc p) n -> p kc n", p=128)

    S = Sched(nc)
    with contextlib.ExitStack() as st:
        _nm = [0]

        def sbt(stack, name, shape, dt):
            _nm[0] += 1
            return stack.enter_context(nc.sbuf_tensor("sb%d_%s" % (_nm[0], name), shape, dt))

        hT_t = sbt(st, "hT", [128, NTT, KC, TT], BF16)
        HT = [Buf("hT%d" % i) for i in range(NTT)]

        def hT(tt, kc, lo=0, hi=TT):
            return hT_t[:, tt, kc, lo:hi]

        NSLOT = 6
        slots = []
        for i in range(NSLOT):
            t = sbt(st, "ws%d" % i, [128, 4096], BF16)
            slots.append((t, [Buf("ws%d_%d" % (i, k)) for k in range(4)], i))
        cf_t = sbt(st, "cf", [128, NCF], F32)
        cb_t = sbt(st, "cb", [128, NCB], BF16)
        vec_t = sbt(st, "vec", [128, DEPTH * NV_L], F32)
        gqs_t = sbt(st, "gqs", [128, DEPTH], F32)
        CF, CB, VEC, GQS = Buf("cf"), Buf("cb"), Buf("vec"), Buf("gqs")
        ident_f = cf_t[:, CF_ID:CF_ID + 128]
        triN = cf_t[:, CF_TRI:CF_TRI + 128]
        tri2N = cf_t[:, CF_TRI2:CF_TRI2 + 128]
        maskBD = cf_t[:, CF_MBD:CF_MBD + 128]
        ones_b = cb_t[:, CB_ONE:CB_ONE + 128]
        ident_b = cb_t[:, CB_ID:CB_ID + 128]
        cmask = cb_t[:, CB_CM:CB_CM + 256]
        XB = [Buf("xb0"), Buf("xb1")]
        sq_t = sbt(st, "sq", [128, KC, TT], BF16)
        SQ = Buf("sq")
        rt_t = [sbt(st, "rt%d" % i, [128, TT], F32) for i in range(2)]
        RT = [Buf("rt0"), Buf("rt1")]
        rs_t = [sbt(st, "rs%d" % i, [128, TT], F32) for i in range(2)]
        RS = [Buf("rs0"), Buf("rs1")]
        cnt = {"rt": 0, "big": 0, "half": 0, "any": 0}

        pb = [st.enter_context(nc.psum_tensor("pb%d" % i, [128, 512], F32)) for i in range(8)]
        BIG = [(Buf("big%d" % i, excl=True), pb[i]) for i in range(4)]
        HALF = [(Buf("sm%d" % k, excl=True), pb[4 + k], 0) for k in range(4)]

        ALLB = BIG + [(hb, ht) for (hb, ht, _o) in HALF]

        def big():
            b, t = BIG[cnt["big"] % 4]
            cnt["big"] += 1
            return PV(b), t

        def any8():
            b, t = ALLB[cnt["any"] % 8]
            cnt["any"] += 1
            return PV(b), t

        def half():
            b, t, off = HALF[cnt["half"] % 4]
            cnt["half"] += 1
            return PV(b), (lambda lo, hi, p0=0, p1=128, t=t, off=off: t[p0:p1, off + lo:off + hi])

        def MM(psbuf, items, reads):
            def fn(e, items=items):
                ins = None
                for (o, l, r, s0, s1) in items:
                    ins = e.matmul(o, lhsT=l, rhs=r, start=s0, stop=s1)
                return ins
            S.op("pe", fn, reads=reads, writes=[psbuf])

        def ACTF(out, in_, func, reads, writes, **kw):
            S.op("act", lambda e: e.activation(out=out, in_=in_, func=func, **kw), reads=reads, writes=writes)

        def TT_(out, in0, in1, op, reads, writes, eng="dve"):
            S.op(eng, lambda e: e.tensor_tensor(out=out, in0=in0, in1=in1, op=op), reads=reads, writes=writes)

        def STT(out, in0, scalar, in1, op0, op1, reads, writes, **kw):
            S.op("dve", lambda e: e.scalar_tensor_tensor(out=out, in0=in0, scalar=scalar, in1=in1, op0=op0, op1=op1, **kw),
                 reads=reads, writes=writes)

        def TS(out, in0, s1, s2, op0, op1, reads, writes, eng="dve"):
            if op1 is None:
                S.op(eng, lambda e: e.tensor_scalar(out=out, in0=in0, scalar1=s1, scalar2=None, op0=op0), reads=reads, writes=writes)
            else:
                S.op(eng, lambda e: e.tensor_scalar(out=out, in0=in0, scalar1=s1, scalar2=s2, op0=op0, op1=op1), reads=reads, writes=writes)

        def RSQRT(ps_ap, psbuf, inv_n):
            i = cnt["rt"] % 2
            cnt["rt"] += 1
            ACTF(rt_t[i][:, :], ps_ap, AF.Ln, [psbuf, EPSB], [RT[i]], bias=EPS_AP, scale=inv_n)
            ACTF(rs_t[i][:, :], rt_t[i][:, :], AF.Exp, [RT[i]], [RS[i]], scale=-0.5)
            return RS[i], rs_t[i]

        class WStream:
            def __init__(self):
                self.plan = []
                self.rec = 0
                self.free = [True] * NSLOT
                self.nxt = 0

            def add(self, loads):
                self.plan.append(loads)

            def pump(self):
                while self.rec < len(self.plan) and self.free[self.rec % NSLOT]:
                    t, subs, si = slots[self.rec % NSLOT]
                    for (sidx, outf, in_ap) in self.plan[self.rec]:
                        S.dma("pool", outf(t), in_ap, writes=[subs[k] for k in sidx], key="w%d_%d" % (si, sidx[0]))
                    self.free[self.rec % NSLOT] = False
                    self.rec += 1

            def get(self):
                i = self.nxt
                self.nxt += 1
                self.pump()
                assert self.rec > i, "weight stream stalled"
                t, subs, si = slots[i % NSLOT]
                return i, t, subs

            def release(self, i):
                self.free[i % NSLOT] = True
                self.pump()

        ws = WStream()

        def v3(t, lo, k, n):
            return t[:, lo:lo + k * n].rearrange("p (k n) -> p k n", k=k)

        eps_t = sbt(st, "eps", [128, 1], F32)
        EPSB = Buf("eps")
        EPS_AP = eps_t[:, 0:1]
        S.op("dve", lambda e: e.memset(eps_t[:, :], EPS), writes=[EPSB])
        one_t = sbt(st, "one", [128, 1], F32)
        ONEB = Buf("one")
        ONE_AP = one_t[:, 0:1]
        S.op("dve", lambda e: e.memset(one_t[:, :], 1.0), writes=[ONEB])
        S.dma("sp", cf_t[:, :], cf_d[:, :], writes=[CF], key="cf")
        S.dma("pool", cb_t[:, :], cb_d[:, :], writes=[CB], key="cb")
        S.dma("sp", vec_t[:, :], vecs_d[:, :], writes=[VEC], key="vec")
        for l in range(DEPTH):
            c = l * NV_L + 32
            TS(gqs_t[:, l:l + 1], vec_t[:, c:c + 1], 128.0 ** -0.5, None, ALU.mult, None, [VEC], [GQS])
        _orig_ACTF = ACTF

        def norm_to_hT(xb, x_ap, gcol, tt, big=None):
            big = big or any8
            xbl = xb if isinstance(xb, list) else [xb]
            ACTF(sq_t[:, :, :], x_ap, AF.Square, xbl, [SQ])
            pbuf, pt = big()
            MM(pbuf, [(pt[:, :], ones_b, sq_t[:, kc, :], kc == 0, kc == KC - 1) for kc in range(KC)], [CB, SQ])
            rb, rt = RSQRT(pt[:, :], pbuf, 1.0 / D)

            def fn(e):
                ins = None
                for kc in range(KC):
                    ins = e.scalar_tensor_tensor(out=hT(tt, kc), in0=x_ap[:, kc, :], scalar=vec_t[:, gcol + kc:gcol + kc + 1],
                                                 in1=rt[:, :], op0=ALU.mult, op1=ALU.mult)
                return ins
            S.op("dve", fn, reads=xbl + [VEC, rb], writes=[HT[tt]])

        XS = [Buf("xs%d" % i) for i in range(NTT)]
        OA = {}
        OB = {}

        with contextlib.ExitStack() as ph0:
            xbuf_t = [sbt(ph0, "xb%d" % i, [128, KC, TT], F32) for i in range(2)]
            for tt in range(NTT):
                i = tt % 2
                S.dma("sp", xbuf_t[i][:, :, :], kcp(xT_d)[:, :, tt * TT:(tt + 1) * TT], writes=[XB[i]], key="x%d" % i)
                norm_to_hT(XB[i], xbuf_t[i][:, :, :], 0 * NV_L + 0, tt)


        def plan_G(l):
            wl = kcp(w_in_d[l])
            ws.add([([0, 1, 2, 3], lambda t: v3(t, 0, 8, 512), wl[:, :, O_GQ:O_GQ + 512])])
            ws.add([([0, 1, 2, 3], lambda t: v3(t, 0, 8, 512), wl[:, :, O_GK:O_GK + 512])])
            ws.add([([0, 1, 2, 3], lambda t: v3(t, 0, 8, 512), wl[:, :, O_GV:O_GV + 512])])
            ws.add([([0, 1, 2, 3], lambda t: v3(t, 0, 8, 512), wl[:, :, O_GV + 512:O_GV + 1024])])
            ws.add([([0, 1, 2, 3], lambda t: v3(t, 0, 8, 512), wl[:, :, O_GR:O_GR + 512])])
            ws.add([([0, 1, 2, 3], lambda t: v3(t, 0, 8, 512), wl[:, :, O_GR + 512:O_GR + 1024])])

        def phase_G(l):
            with contextlib.ExitStack() as ph:
                glr_t = sbt(ph, "glrT", [128, TT], BF16)
                GLR = Buf("glr")
                GLRC = Buf("glrc")
                w2_t = sbt(ph, "w2aug", [128, 512], BF16)
                W2 = Buf("w2")
                wglr_t = sbt(ph, "wglr", [128, KC, 16], BF16)
                WGLR = Buf("wglr")
                qs_t = sbt(ph, "qTs", [128, 4, TT], F32)
                ks_t = sbt(ph, "kTs", [128, 4, TT], F32)
                QS = [Buf("qs%d" % h) for h in range(4)]
                KS = [Buf("ks%d" % h) for h in range(4)]
                oacc_t = sbt(ph, "oacc", [128, 8, TT], F32)
                OACC = [Buf("oacc%d" % c) for c in range(8)]
                Sf_t = sbt(ph, "Sf", [128, 4, 2, 256], F32)
                Sb_t = sbt(ph, "Sb", [128, 4, 2, 256], BF16)
                SF = [[Buf("sf%d%d" % (h, i)) for i in range(2)] for h in range(4)]
                SBB = [[Buf("sb%d%d" % (h, i)) for i in range(2)] for h in range(4)]
                e1_t = sbt(ph, "e1", [128, 4, 128], F32)
                l1_t = e1_t
                E1_t = sbt(ph, "E1", [128, 4, 128], F32)
                E2_t = sbt(ph, "E2", [128, 4, 128], F32)
                E3_t = sbt(ph, "E3", [128, 4, 128], F32)
                qd_t = sbt(ph, "qd", [128, 4, 128], BF16)
                kd_t = sbt(ph, "kd", [128, 4, 128], BF16)
                kl0_t = sbt(ph, "kl0", [128, 4, 128], BF16)
                kl1_t = sbt(ph, "kl1", [128, 4, 128], BF16)
                vs_t = sbt(ph, "vs", [128, 4, 256], BF16)
                atm_t = sbt(ph, "atm", [128, 4, 128], BF16)
                Eb = [Buf("e1_%d" % h) for h in range(4)]
                Lb = [Buf("l1_%d" % h) for h in range(4)]
                E1b = [Buf("E1_%d" % h) for h in range(4)]
                E2b = [Buf("E2_%d" % h) for h in range(4)]
                E3b = [Buf("E3_%d" % h) for h in range(4)]
                QD = [Buf("qd%d" % h) for h in range(4)]
                KD = [Buf("kd%d" % h) for h in range(4)]
                KL0 = [Buf("kl0%d" % h) for h in range(4)]
                KL1 = [Buf("kl1%d" % h) for h in range(4)]
                VS = [Buf("vs%d" % h) for h in range(4)]
                ATM = [Buf("atm%d" % h) for h in range(4)]
                sqo_t = sbt(ph, "sqo", [128, 2, TT], BF16)
                SQO = Buf("sqo")
                gs_t = [sbt(ph, "gs%d" % i, [128, TT], F32) for i in range(2)]
                GS = [Buf("gs0"), Buf("gs1")]
                oao_t = [sbt(ph, "oao%d" % i, [128, TT], BF16) for i in range(3)]
                OAO = [Buf("oao%d" % i) for i in range(3)]

                if dbg is not None:
                    dbg.update(dict(glr=glr_t, e1=e1_t, E1=E1_t, E2=E2_t, E3=E3_t, qd=qd_t, kd=kd_t, kl0=kl0_t, kl1=kl1_t,
                                    vs=vs_t, atm=atm_t, Sf=Sf_t, Sb=Sb_t, qs=qs_t, ks=ks_t, oacc=oacc_t, w2=w2_t))
                S.op("dve", lambda e: e.memset(glr_t[:, :], 0.0), writes=[GLRC])
                S.op("dve", lambda e: e.memset(glr_t[0:32, :], 1.0), writes=[GLRC])
                S.op("dve", lambda e: e.memset(kl0_t[:, :, :], 0.0), writes=KL0)
                S.op("dve", lambda e: e.memset(kl1_t[:, :, :], 0.0), writes=KL1)
                S.op("dve", lambda e: e.memset(Sf_t[:, :, :, :], 0.0), writes=[SF[h][0] for h in range(4)])
                S.op("dve", lambda e: e.memset(Sb_t[:, :, :, :], 0.0), writes=[SBB[h][0] for h in range(4)])
                S.dma("pool", w2_t[:, :], w2aug_d[l], writes=[W2], key="w2aug")
                S.dma("pool", wglr_t[:, :, :], kcp(w_in_d[l])[:, :, O_GLR:O_GLR + 16], writes=[WGLR], key="wglr")
                iq, tq, sq_ = ws.get()
                ik, tk, sk_ = ws.get()
                iv0, tv0, sv0 = ws.get()
                iv1, tv1, sv1 = ws.get()
                ir0, tr0, sr0 = ws.get()
                ir1, tr1, sr1 = ws.get()
                wq = v3(tq, 0, 8, 512)
                wk = v3(tk, 0, 8, 512)
                wv = [v3(tv0, 0, 8, 512), v3(tv1, 0, 8, 512)]
                wr = [v3(tr0, 0, 8, 512), v3(tr1, 0, 8, 512)]
                svs = [sv0, sv1]
                srs = [sr0, sr1]

                oa_i = 0
                for tt in range(NTT):
                    pbuf, pt = big()
                    MM(pbuf, [(pt[0:16, :], wglr_t[:, kc, :], hT(tt, kc), kc == 0, kc == KC - 1) for kc in range(KC)],
                       [WGLR, HT[tt]])
                    ACTF(glr_t[0:16, :], pt[0:16, :], AF.Copy, [pbuf, GLRC], [GLR])
                    for h in range(4):
                        pbuf, pt = big()
                        MM(pbuf, [(pt[:, :], wq[:, kc, h * 128:(h + 1) * 128], hT(tt, kc), kc == 0, kc == KC - 1) for kc in range(KC)],
                           sq_ + [HT[tt]])
                        ACTF(qs_t[:, h, :], pt[:, :], AF.Copy, [pbuf], [QS[h]], scale=128.0 ** -0.5)
                        pbuf, pt = big()
                        MM(pbuf, [(pt[:, :], wk[:, kc, h * 128:(h + 1) * 128], hT(tt, kc), kc == 0, kc == KC - 1) for kc in range(KC)],
                           sk_ + [HT[tt]])
                        S.op("dve", lambda e, h=h, pt=pt: e.tensor_copy(out=ks_t[:, h, :], in_=pt[:, :]), reads=[pbuf], writes=[KS[h]])
                    for stt in range(4):
                        t0 = stt * 128
                        for h in range(4):
                            b_, f_ = half()
                            MM(b_, [(f_(0, 128), glr_t[:, t0:t0 + 128], w2_t[:, h * 128:(h + 1) * 128], True, True)], [GLR, GLRC, W2])
                            ACTF(e1_t[:, h, :], f_(0, 128), AF.Exp, [b_], [Eb[h]], scale=-1.0)
                        for h in range(4):
                            b_, f_ = half()
                            MM(b_, [(f_(0, 256), hT(tt, kc, t0, t0 + 128), wv[h // 2][:, kc, (h % 2) * 256:(h % 2) * 256 + 256], kc == 0, kc == KC - 1)
                                    for kc in range(KC)], svs[h // 2] + [HT[tt]])
                            ACTF(vs_t[:, h, :], f_(0, 256), AF.Copy, [b_], [VS[h]])
                        ktp = []
                        for h in range(4):
                            b_, pt = big()
                            MM(b_, [(pt[:, 0:128], hT(tt, kc, t0, t0 + 128), wk[:, kc, h * 128:(h + 1) * 128], kc == 0, kc == KC - 1)
                                    for kc in range(KC)], sk_ + [HT[tt]])
                            ktp.append((b_, pt))
                        for h in range(4):
                            ACTF(l1_t[:, h, :], e1_t[:, h, :], AF.Ln, [Eb[h], ONEB], [Eb[h]], bias=ONE_AP)
                        for h in range(4):
                            b_, f_ = half()
                            MM(b_, [(f_(0, 128), l1_t[:, h, :], triN, True, True)], [Eb[h], CF])
                            ACTF(E1_t[:, h, :], f_(0, 128), AF.Exp, [b_], [E1b[h]])
                            ACTF(E2_t[:, h, :], f_(0, 128), AF.Exp, [b_], [E2b[h]], scale=-1.0)
                            b2, f2 = half()
                            MM(b2, [(f2(0, 128), tri2N, l1_t[:, h, :], True, True)], [Eb[h], CF])
                            ACTF(E3_t[:, h, :], f2(0, 128), AF.Exp, [b2], [E3b[h]])
                        for h in range(4):
                            b_, pt = ktp[h]
                            TT_(kl0_t[0:64, h, :], pt[0:64, 0:128], E3_t[0:64, h, :], ALU.mult, [b_, E3b[h]], [KL0[h]])
                            TT_(kl1_t[64:128, h, :], pt[64:128, 0:128], E3_t[64:128, h, :], ALU.mult, [b_, E3b[h]], [KL1[h]])
                            TT_(qd_t[:, h, :], qs_t[:, h, t0:t0 + 128], E1_t[:, h, :], ALU.mult, [QS[h], E1b[h]], [QD[h]])
                            TT_(kd_t[:, h, :], ks_t[:, h, t0:t0 + 128], E2_t[:, h, :], ALU.mult, [KS[h], E2b[h]], [KD[h]])
                        for h in range(4):
                            b_, f_ = half()
                            MM(b_, [(f_(0, 128), kd_t[:, h, :], qd_t[:, h, :], True, True)], [KD[h], QD[h]])
                            TT_(atm_t[:, h, :], f_(0, 128), maskBD, ALU.mult, [b_, CF], [ATM[h]])
                        for h in range(4):
                            b_, f_ = half()
                            MM(b_, [(f_(0, 256), kl0_t[:, h, :], vs_t[:, h, :], True, True)], [KL0[h], VS[h]])
                            STT(Sf_t[:, h, 1, :], Sf_t[:, h, 0, :], E1_t[:, h, 63:64], f_(0, 256), ALU.mult, ALU.add,
                                [SF[h][0], E1b[h], b_], [SF[h][1]])
                            ACTF(Sb_t[:, h, 1, :], Sf_t[:, h, 1, :], AF.Copy, [SF[h][1]], [SBB[h][1]])
                        for h in range(4):
                            for dvc in range(2):
                                b_, f_ = half()
                                MM(b_, [(f_(0, 128), vs_t[:, h, dvc * 128:(dvc + 1) * 128], atm_t[:, h, :], True, False),
                                        (f_(0, 64), Sb_t[:, h, 0, dvc * 128:(dvc + 1) * 128], qd_t[:, h, 0:64], False, False),
                                        (f_(64, 128), Sb_t[:, h, 1, dvc * 128:(dvc + 1) * 128], qd_t[:, h, 64:128], False, True)],
                                   [VS[h], ATM[h], SBB[h][0], SBB[h][1], QD[h]])
                                c = h * 2 + dvc
                                if dvc == 0:
                                    ACTF(oacc_t[:, c, t0:t0 + 128], f_(0, 128), AF.Copy, [b_], [OACC[c]])
                                else:
                                    S.op("dve", lambda e, c=c, f_=f_, t0=t0: e.tensor_copy(out=oacc_t[:, c, t0:t0 + 128], in_=f_(0, 128)),
                                         reads=[b_], writes=[OACC[c]])
                        for h in range(4):
                            b_, f_ = half()
                            MM(b_, [(f_(0, 256), kl1_t[:, h, :], vs_t[:, h, :], True, True)], [KL1[h], VS[h]])
                            STT(Sf_t[:, h, 0, :], Sf_t[:, h, 1, :], E1_t[:, h, 127:128], f_(0, 256), ALU.mult, ALU.add,
                                [SF[h][1], E1b[h], b_], [SF[h][0]])
                            ACTF(Sb_t[:, h, 0, :], Sf_t[:, h, 0, :], AF.Copy, [SF[h][0]], [SBB[h][0]])
                    for h in range(4):
                        ACTF(sqo_t[:, :, :], oacc_t[:, 2 * h:2 * h + 2, :], AF.Square, [OACC[2 * h], OACC[2 * h + 1]], [SQO])
                        pbuf, pt = big()
                        MM(pbuf, [(pt[:, :], ones_b, sqo_t[:, 0, :], True, False), (pt[:, :], ones_b, sqo_t[:, 1, :], False, True)], [CB, SQO])
                        rb, rt = RSQRT(pt[:, :], pbuf, 1.0 / 256.0)
                        for dvc in range(2):
                            c = h * 2 + dvc
                            pbuf, pt = big()
                            MM(pbuf, [(pt[:, :], wr[c // 4][:, kc, (c % 4) * 128:(c % 4) * 128 + 128], hT(tt, kc), kc == 0, kc == KC - 1)
                                      for kc in range(KC)], srs[c // 4] + [HT[tt]])
                            gi = c % 2
                            ACTF(gs_t[gi][:, :], pt[:, :], AF.Silu, [pbuf], [GS[gi]])
                            gcol = l * NV_L + 24 + c
                            STT(oacc_t[:, c, :], oacc_t[:, c, :], vec_t[:, gcol:gcol + 1], rt[:, :], ALU.mult, ALU.mult,
                                [OACC[c], VEC, rb], [OACC[c]])
                            oi = oa_i % 3
                            oa_i += 1
                            TT_(oao_t[oi][:, :], oacc_t[:, c, :], gs_t[gi][:, :], ALU.mult, [OACC[c], GS[gi]], [OAO[oi]])
                            OA[(c, tt)] = Buf("oa%d_%d" % (c, tt))
                            S.dma("sp", oaT_d[c, :, tt * TT:(tt + 1) * TT], oao_t[oi][:, :], reads=[OAO[oi]], writes=[OA[(c, tt)]],
                                  key="oast%d" % oi)
                for i in (iq, ik, iv0, iv1, ir0, ir1):
                    ws.release(i)
            S.barrier()

        def plan_M(l):
            wl = kcp(w_in_d[l])
            for h in range(8):
                ws.add([([0], lambda t: v3(t, 0, 8, 128), wl[:, :, O_MQ + h * 128:O_MQ + (h + 1) * 128]),
                        ([1], lambda t: v3(t, 1024, 8, 128), wl[:, :, O_MK + h * 128:O_MK + (h + 1) * 128]),
                        ([2], lambda t: v3(t, 2048, 8, 128), wl[:, :, O_MV + h * 128:O_MV + (h + 1) * 128])])

        def phase_M(l):
            with contextlib.ExitStack() as ph:
                laug_t = sbt(ph, "laug", [128, 32, 128], BF16)
                LAUG = Buf("laug")
                QT_t = sbt(ph, "QT", [128, NT], BF16)
                KT_t = sbt(ph, "KT", [128, NT], BF16)
                V_t = sbt(ph, "Vp", [128, 32, 129], BF16)
                raug_t = sbt(ph, "raug", [128, NT], BF16)
                obt_t = sbt(ph, "obt", [128, NT], BF16)
                QTB = [Buf("qt%d" % i) for i in range(NTT)]
                KTB = [Buf("kt%d" % i) for i in range(NTT)]
                VB = [Buf("v%d" % i) for i in range(32)]
                VONE = Buf("vone")
                RA = [Buf("ra%d" % i) for i in range(16)]
                RAZ = Buf("raz")
                RAC = Buf("rac")
                OBB = [Buf("obb%d" % i) for i in range(16)]
                ksum_t = sbt(ph, "ksum", [128, 16], F32)
                KSUM = [Buf("ksum%d" % i) for i in range(16)]
                kmT_t = sbt(ph, "kmT", [128, 16], BF16)
                KMT = Buf("kmt")
                qf_t = [sbt(ph, "qf%d" % i, [128, TT], F32) for i in range(2)]
                QF = [Buf("qf0"), Buf("qf1")]
                sqq_t = [sbt(ph, "sqq%d" % i, [128, TT], BF16) for i in range(2)]
                SQQ = [Buf("sqq0"), Buf("sqq1")]
                g1_t = sbt(ph, "g1", [128, 512], F32)
                m8_t = sbt(ph, "m8", [128, 256], F32)
                tg_t = sbt(ph, "tg", [128, 512], F32)
                mb_t = sbt(ph, "mb", [128, 512], F32)
                G1, M8, TG, MB = Buf("g1"), Buf("m8"), Buf("tg"), Buf("mb")
                NP = 4
                pT_t = [sbt(ph, "pT%d" % i, [128, 512], BF16) for i in range(NP)]
                PT = [Buf("pT%d" % i) for i in range(NP)]
                rd_t = [sbt(ph, "rd%d" % i, [128, 1], F32) for i in range(4)]
                RD = [Buf("rd%d" % i) for i in range(4)]
                on_t = [sbt(ph, "on%d" % i, [128, 128], F32) for i in range(4)]
                ON = [Buf("on%d" % i) for i in range(4)]

                S.dma("pool", laug_t[:, :, :], laug_d.rearrange("p (v k) -> p v k", v=32), writes=[LAUG], key="laug")
                S.op("dve", lambda e: e.memset(V_t[:, :, :], 1.0), writes=[VONE])
                S.op("dve", lambda e: e.memset(raug_t[:, :], 0.0), writes=[RAZ])
                pcnt = 0
                ocnt = 0
                gcnt = 0
                for h in range(8):
                    iw, tw, sw = ws.get()
                    wmq = v3(tw, 0, 8, 128)
                    wmk = v3(tw, 1024, 8, 128)
                    wmv = v3(tw, 2048, 8, 128)
                    S.dma("pool", raug_t[16:18, :], augc_d[h], reads=[RAZ], writes=[RAC], key="raugc")
                    gq_ap = gqs_t[:, l:l + 1]
                    gk_ap = vec_t[:, l * NV_L + 33:l * NV_L + 34]
                    for tt in range(NTT):
                        pq, ptq = big()
                        MM(pq, [(ptq[:, :], wmq[:, kc, :], hT(tt, kc), kc == 0, kc == KC - 1) for kc in range(KC)], [sw[0], HT[tt]])
                        pk, ptk = big()
                        MM(pk, [(ptk[:, :], wmk[:, kc, :], hT(tt, kc), kc == 0, kc == KC - 1) for kc in range(KC)], [sw[1], HT[tt]])
                        ACTF(sqq_t[0][:, :], ptq[:, :], AF.Square, [pq], [SQQ[0]])
                        ACTF(qf_t[0][:, :], ptq[:, :], AF.Copy, [pq], [QF[0]])
                        ACTF(sqq_t[1][:, :], ptk[:, :], AF.Square, [pk], [SQQ[1]])
                        ACTF(qf_t[1][:, :], ptk[:, :], AF.Copy, [pk], [QF[1]])
                        pb2, pt2 = big()
                        MM(pb2, [(pt2[:, :], ones_b, sqq_t[0][:, :], True, True)], [CB, SQQ[0]])
                        rb, rt = RSQRT(pt2[:, :], pb2, 1.0 / 128.0)
                        STT(QT_t[:, tt * TT:(tt + 1) * TT], qf_t[0][:, :], gq_ap, rt[:, :], ALU.mult, ALU.mult,
                            [QF[0], GQS, rb], [QTB[tt]])
                        for stt in range(4):
                            ti = tt * 4 + stt
                            b_, f_ = half()
                            MM(b_, [(f_(0, 128), hT(tt, kc, stt * 128, stt * 128 + 128), wmv[:, kc, :], kc == 0, kc == KC - 1)
                                    for kc in range(KC)], [sw[2], HT[tt]])
                            S.op("dve", lambda e, f_=f_, ti=ti: e.tensor_copy(out=V_t[:, ti, 0:128], in_=f_(0, 128)), reads=[b_, VONE], writes=[VB[ti]])
                        pb3, pt3 = big()
                        MM(pb3, [(pt3[:, :], ones_b, sqq_t[1][:, :], True, True)], [CB, SQQ[1]])
                        rb, rt = RSQRT(pt3[:, :], pb3, 1.0 / 128.0)
                        for blk in range(2):
                            bi = tt * 2 + blk
                            lo = blk * 256
                            STT(KT_t[:, tt * TT + lo:tt * TT + lo + 256], qf_t[1][:, lo:lo + 256], gk_ap, rt[:, lo:lo + 256],
                                ALU.mult, ALU.mult, [QF[1], VEC, rb], [KTB[tt], KSUM[bi]], accum_out=ksum_t[:, bi:bi + 1])
                    ws.release(iw)
                    TS(kmT_t[:, :], ksum_t[:, :], 1.0 / 256.0, None, ALU.mult, None, KSUM, [KMT])
                    gb_, gt_ = big()
                    MM(gb_, [(gt_[:, qt * 16:(qt + 1) * 16], QT_t[:, qt * 128:(qt + 1) * 128], kmT_t[:, :], True, True) for qt in range(32)],
                       QTB + [KMT])
                    TT_(g1_t[:, :], gt_[:, :], cf_t[:, CF_GM:CF_GM + 512], ALU.add, [gb_, CF], [G1])

                    def fmax(e):
                        ins = None
                        for qt in range(32):
                            ins = e.max(out=m8_t[:, qt * 8:(qt + 1) * 8], in_=g1_t[:, qt * 16:(qt + 1) * 16])
                        return ins
                    S.op("dve", fmax, reads=[G1], writes=[M8])

                    def fthr(e):
                        ins = None
                        for qt in range(32):
                            ins = e.tensor_scalar(out=tg_t[:, qt * 16:(qt + 1) * 16], in0=g1_t[:, qt * 16:(qt + 1) * 16],
                                                  scalar1=m8_t[:, qt * 8 + 3:qt * 8 + 4], scalar2=NEG, op0=ALU.is_lt, op1=ALU.mult)
                        return ins
                    S.op("dve", fthr, reads=[G1, M8], writes=[TG])
                    STT(mb_t[:, :], cf_t[:, CF_AB:CF_AB + 512], slopes[h], tg_t[:, :], ALU.mult, ALU.add, [CF, TG], [MB])
                    for g in range(8):
                        b2, t2 = big()

                        def ftr(e, g=g, t2=t2):
                            ins = None
                            for k in range(4):
                                qt = g * 4 + k
                                ins = e.transpose(out=t2[0:16, k * 128:(k + 1) * 128], in_=mb_t[:, qt * 16:(qt + 1) * 16], identity=ident_f)
                            return ins
                        S.op("pe", ftr, reads=[MB, CF], writes=[b2])
                        ACTF(raug_t[0:16, g * 512:(g + 1) * 512], t2[0:16, :], AF.Copy, [b2, RAZ], [RA[2 * g], RA[2 * g + 1]])
                    pending = []

                    def flush_epilogue():
                        for (ons, q0_, b_i) in pending:
                            for hf, oi in enumerate(ons):
                                b2, t2 = big()
                                S.op("pe", lambda e, t2=t2, oi=oi: e.transpose(out=t2[:, 0:128], in_=on_t[oi][:, :], identity=ident_f),
                                     reads=[ON[oi], CF], writes=[b2])
                                S.op("dve", lambda e, t2=t2, q0_=q0_, hf=hf: e.tensor_copy(out=obt_t[:, q0_ + hf * 128:q0_ + hf * 128 + 128], in_=t2[:, 0:128]),
                                     reads=[b2], writes=[OBB[b_i]])
                        del pending[:]

                    for b in range(16):
                        q0 = b * 256
                        oAb, oAt = big()
                        oBb, oBt = big()
                        units = list(range(b + 1))
                        n = len(units)
                        sps = [None] * n
                        firstA = [True]
                        firstB = [True]

                        def issue_S(i):
                            j = units[i]
                            b_, f_ = half()
                            k0 = j * 256
                            rds = [KTB[k0 // TT], QTB[q0 // TT], LAUG, RA[b], RAC, RAZ, CB]
                            if j < b:
                                items = []
                                for c in range(2):
                                    items += [(f_(c * 256, c * 256 + 256), KT_t[:, k0 + c * 128:k0 + c * 128 + 128], QT_t[:, q0:q0 + 256], True, False),
                                              (f_(c * 256, c * 256 + 256), laug_t[:, j * 2 + c, :], raug_t[:, q0:q0 + 256], False, True)]
                            else:
                                items = [(f_(0, 256), KT_t[:, k0:k0 + 128], QT_t[:, q0:q0 + 256], True, False),
                                         (f_(0, 256), laug_t[:, j * 2, :], raug_t[:, q0:q0 + 256], False, False),
                                         (f_(0, 256), ident_b, cmask, False, True),
                                         (f_(256, 384), KT_t[:, k0 + 128:k0 + 256], QT_t[:, q0 + 128:q0 + 256], True, False),
                                         (f_(256, 384), laug_t[:, j * 2 + 1, :], raug_t[:, q0 + 128:q0 + 256], False, False),
                                         (f_(256, 384), ident_b, cmask[:, 0:128], False, True)]
                            MM(b_, items, rds)
                            sps[i] = (b_, f_)

                        issue_S(0)
                        if n > 1:
                            issue_S(1)
                        for i in range(n):
                            if i + 2 < n:
                                issue_S(i + 2)
                            j = units[i]
                            b_, f_ = sps[i]
                            pi = pcnt % NP
                            pcnt += 1
                            own = (j == b)
                            w = 384 if own else 512
                            ACTF(pT_t[pi][:, 0:w], f_(0, w), AF.Exp, [b_], [PT[pi]])
                            rdsA = [PT[pi], VB[j * 2], VONE]
                            rdsB = [PT[pi], VB[j * 2], VB[j * 2 + 1], VONE]
                            if not own:
                                MM(oAb, [(oAt[:, 0:129], pT_t[pi][:, 0:128], V_t[:, j * 2, :], firstA[0], False),
                                         (oAt[:, 0:129], pT_t[pi][:, 256:384], V_t[:, j * 2 + 1, :], False, False)], rdsB)
                                MM(oBb, [(oBt[:, 0:129], pT_t[pi][:, 128:256], V_t[:, j * 2, :], firstB[0], False),
                                         (oBt[:, 0:129], pT_t[pi][:, 384:512], V_t[:, j * 2 + 1, :], False, False)], rdsB)
                            else:
                                MM(oAb, [(oAt[:, 0:129], pT_t[pi][:, 0:128], V_t[:, j * 2, :], firstA[0], True)], rdsA)
                                MM(oBb, [(oBt[:, 0:129], pT_t[pi][:, 128:256], V_t[:, j * 2, :], firstB[0], False),
                                         (oBt[:, 0:129], pT_t[pi][:, 256:384], V_t[:, j * 2 + 1, :], False, True)], rdsB)
                            firstA[0] = False
                            firstB[0] = False
                            if i == 0:
                                flush_epilogue()
                        ons = []
                        for hf, (ob_, ot_) in enumerate(((oAb, oAt), (oBb, oBt))):
                            oi = ocnt % 4
                            ocnt += 1
                            S.op("dve", lambda e, oi=oi, ot_=ot_: e.reciprocal(out=rd_t[oi][:, :], in_=ot_[:, 128:129]), reads=[ob_], writes=[RD[oi]])
                            TS(on_t[oi][:, :], ot_[:, 0:128], rd_t[oi][:, 0:1], None, ALU.mult, None, [ob_, RD[oi]], [ON[oi]])
                            ons.append(oi)
                        pending.append((ons, q0, b))
                    flush_epilogue()
                    OB[h] = Buf("ob%d" % h)
                    S.dma("sp", obT_d[h, :, :], obt_t[:, :], reads=OBB, writes=[OB[h]], key="obst")
            S.barrier()

        def plan_T(l):
            wl = kcp(w_in_d[l])
            for tt in range(NTT):
                for oc in range(8):
                    cs = slice(oc * 128, (oc + 1) * 128)
                    ws.add([([0], lambda t: v3(t, 0, 8, 128), kcp(wa_d[l])[:, :, cs]),
                            ([1], lambda t: v3(t, 1024, 8, 128), wl[:, :, O_GA + oc * 128:O_GA + (oc + 1) * 128]),
                            ([2], lambda t: v3(t, 2048, 8, 128), kcp(wb_d[l])[:, :, cs]),
                            ([3], lambda t: v3(t, 3072, 8, 128), wl[:, :, O_GB + oc * 128:O_GB + (oc + 1) * 128])])
                for og in range(2):
                    ws.add([([k], (lambda t, k=k: v3(t, k * 1024, 8, 128)), kcp(wo_d[l])[:, :, (og * 4 + k) * 128:(og * 4 + k + 1) * 128])
                            for k in range(4)])
                for fh in range(2):
                    for fg in range(4):
                        c0 = fh * 2048 + fg * 512
                        ws.add([([0, 1, 2, 3], lambda t: v3(t, 0, 8, 512), kcp(wup_d[l])[:, :, c0:c0 + 512])])
                    for oc in range(8):
                        ws.add([([0, 1], lambda t: v3(t, 0, 16, 128),
                                 kcp(wdn_d[l][fh * 2048:(fh + 1) * 2048, :])[:, :, oc * 128:(oc + 1) * 128])])
                for og in range(4):
                    lo = []
                    for k in range(2):
                        oc = og * 2 + k
                        lo.append(([2 * k], (lambda t, k=k: v3(t, 2 * k * 1024, 8, 128)), kcp(wpg_d[l])[:, :, oc * 128:(oc + 1) * 128]))
                        lo.append(([2 * k + 1], (lambda t, k=k: v3(t, (2 * k + 1) * 1024, 2, 128)), kcp(wple_d[l])[:, :, oc * 128:(oc + 1) * 128]))
                    ws.add(lo)

        def phase_T(l, last):
            with contextlib.ExitStack() as ph:
                xbuf_t = [sbt(ph, "xb%d" % i, [128, KC, TT], F32) for i in range(2)]
                aT_t = sbt(ph, "aT", [128, 24, TT], BF16)
                ATC = [Buf("atc%d" % i) for i in range(24)]
                AT = [ATC[0:8], ATC[8:16], ATC[16:24]]
                XC = [[Buf("xc%d_%d" % (i, c)) for c in range(KC)] for i in range(2)]
                ptb_t = sbt(ph, "ptb", [128, 2, TT], BF16)
                PTB = Buf("ptb")
                sa_t = [sbt(ph, "sa%d" % i, [128, TT], F32) for i in range(2)]
                SA = [Buf("sa0"), Buf("sa1")]
                sbg_t = [sbt(ph, "sbg%d" % i, [128, TT], F32) for i in range(2)]
                SBG = [Buf("sbg0"), Buf("sbg1")]
                rl_t = [sbt(ph, "rl%d" % i, [128, TT], F32) for i in range(2)]
                RL = [Buf("rl0"), Buf("rl1")]
                src_d = xT_d if l == 0 else xS_d
                dst_d = out_d if last else xS_d
                def issue_loads(t_):
                    xi_ = t_ % 2
                    tsl_ = slice(t_ * TT, (t_ + 1) * TT)
                    S.dma("sp", xbuf_t[xi_][:, :, :], kcp(src_d)[:, :, tsl_], reads=([XS[t_]] if l > 0 else []), writes=XC[xi_], key="x%d" % xi_)
                    S.dma("sp", aT_t[:, 0:8, :], oaT_d.rearrange("c p t -> p c t")[:, :, tsl_], reads=[OA[(c, t_)] for c in range(8)],
                          writes=AT[0], key="ata")
                    S.dma("sp", aT_t[:, 8:16, :], obT_d.rearrange("c p t -> p c t")[:, :, tsl_], reads=[OB[h] for h in range(8)],
                          writes=AT[1], key="atb")

                issue_loads(0)
                for tt in range(NTT):
                    xi = tt % 2
                    xb, xt = XC[xi], xbuf_t[xi]
                    tsl = slice(tt * TT, (tt + 1) * TT)
                    S.dma("pool", ptb_t[:, :, :], kcp(pT_d[l])[:, :, tsl], writes=[PTB], key="ptb")
                    for oc in range(8):
                        iw, tw, sw = ws.get()
                        w4 = [v3(tw, k * 1024, 8, 128) for k in range(4)]
                        i2 = oc % 2
                        pa, pat = any8()
                        MM(pa, [(pat[:, :], w4[0][:, kc, :], aT_t[:, kc, :], kc == 0, kc == KC - 1) for kc in range(KC)], [sw[0]] + AT[0])
                        pg, pgt = any8()
                        MM(pg, [(pgt[:, :], w4[1][:, kc, :], hT(tt, kc), kc == 0, kc == KC - 1) for kc in range(KC)], [sw[1], HT[tt]])
                        ACTF(sa_t[i2][:, :], pgt[:, :], AF.Sigmoid, [pg], [SA[i2]])
                        TT_(sa_t[i2][:, :], pat[:, :], sa_t[i2][:, :], ALU.mult, [pa, SA[i2]], [SA[i2]])
                        pb_, pbt = any8()
                        MM(pb_, [(pbt[:, :], w4[2][:, kc, :], aT_t[:, 8 + kc, :], kc == 0, kc == KC - 1) for kc in range(KC)], [sw[2]] + AT[1])
                        pg2, pg2t = any8()
                        MM(pg2, [(pg2t[:, :], w4[3][:, kc, :], hT(tt, kc), kc == 0, kc == KC - 1) for kc in range(KC)], [sw[3], HT[tt]])
                        ws.release(iw)
                        ACTF(sbg_t[i2][:, :], pg2t[:, :], AF.Sigmoid, [pg2], [SBG[i2]])
                        TT_(sbg_t[i2][:, :], pbt[:, :], sbg_t[i2][:, :], ALU.mult, [pb_, SBG[i2]], [SBG[i2]])
                        TT_(aT_t[:, 16 + oc, :], sa_t[i2][:, :], sbg_t[i2][:, :], ALU.add, [SA[i2], SBG[i2]], [ATC[16 + oc]])
                    for og in range(2):
                        iw, tw, sw = ws.get()
                        for k in range(4):
                            oc = og * 4 + k
                            wv_ = v3(tw, k * 1024, 8, 128)
                            pd, pdt = any8()
                            MM(pd, [(pdt[:, :], wv_[:, kc, :], aT_t[:, 16 + kc, :], kc == 0, kc == KC - 1) for kc in range(KC)], [sw[k]] + AT[2])
                            TT_(xt[:, oc, :], xt[:, oc, :], pdt[:, :], ALU.add, [xb[oc], pd], [xb[oc]])
                        ws.release(iw)
                    norm_to_hT(xb, xt[:, :, :], l * NV_L + 8, tt)
                    for fh in range(2):
                        for fg in range(4):
                            iw, tw, sw = ws.get()
                            wu = v3(tw, 0, 8, 512)
                            for k in range(4):
                                ffc = fg * 4 + k
                                i2 = ffc % 2
                                pu, put = any8()
                                MM(pu, [(put[:, :], wu[:, kc, k * 128:(k + 1) * 128], hT(tt, kc), kc == 0, kc == KC - 1) for kc in range(KC)],
                                   sw + [HT[tt]])
                                ACTF(rl_t[i2][:, :], put[:, :], AF.Relu, [pu], [RL[i2]])
                                TT_(aT_t[:, ffc, :], rl_t[i2][:, :], rl_t[i2][:, :], ALU.mult, [RL[i2]], [ATC[ffc]])
                            ws.release(iw)
                        for oc in range(8):
                            iw, tw, sw = ws.get()
                            wd = v3(tw, 0, 16, 128)
                            pd, pdt = any8()
                            MM(pd, [(pdt[:, :], wd[:, kc, :], aT_t[:, kc, :], kc == 0, kc == 15) for kc in range(16)], sw[0:2] + ATC[0:16])
                            ws.release(iw)
                            TT_(xt[:, oc, :], xt[:, oc, :], pdt[:, :], ALU.add, [xb[oc], pd], [xb[oc]])
                    if tt + 1 < NTT:
                        issue_loads(tt + 1)
                    norm_to_hT(xb, xt[:, :, :], l * NV_L + 16, tt)
                    for og in range(4):
                        iw, tw, sw = ws.get()
                        for k in range(2):
                            oc = og * 2 + k
                            i2 = oc % 2
                            wg = v3(tw, 2 * k * 1024, 8, 128)
                            wp = v3(tw, (2 * k + 1) * 1024, 2, 128)
                            pg, pgt = any8()
                            MM(pg, [(pgt[:, :], wg[:, kc, :], hT(tt, kc), kc == 0, kc == KC - 1) for kc in range(KC)], [sw[2 * k], HT[tt]])
                            pe_, pet = any8()
                            MM(pe_, [(pet[:, :], wp[:, kc, :], ptb_t[:, kc, :], kc == 0, kc == 1) for kc in range(2)], [sw[2 * k + 1], PTB])
                            ACTF(sa_t[i2][:, :], pgt[:, :], AF.Sigmoid, [pg], [SA[i2]])
                            TT_(sa_t[i2][:, :], pet[:, :], sa_t[i2][:, :], ALU.mult, [pe_, SA[i2]], [SA[i2]])
                            TT_(xt[:, oc, :], xt[:, oc, :], sa_t[i2][:, :], ALU.add, [xb[oc], SA[i2]], [xb[oc]])
                        ws.release(iw)
                    S.dma("sp", kcp(dst_d)[:, :, tsl], xt[:, :, :], reads=xb, writes=[XS[tt]], key="xst%d" % xi)
                    if not last:
                        norm_to_hT(xb, xt[:, :, :], (l + 1) * NV_L + 0, tt)
            S.barrier()

        S.barrier()

        plan_G(0)
        for l in range(depth):
            last = (l == depth - 1)
            plan_M(l)
            phase_G(l)
            if stop_after == "G":
                break
            plan_T(l)
            phase_M(l)
            if stop_after == "M":
                break
            if not last:
                plan_G(l + 1)
            phase_T(l, last)
        S.op("sp", lambda e: e.nop(), reads=XS)
        S.emit()
    return nc


_NC_CACHE = {}


def _host_layout(inputs):
    cf, cb, laug, augc, _ = host_consts()
    vecs = np.zeros((128, DEPTH * NV_L), np.float32)
    f = lambda a: np.asarray(a, dtype=np.float32)
    nm, nmlp, nple = f(inputs["norm_mix"]), f(inputs["norm_mlp"]), f(inputs["norm_ple"])
    gon, mqn, mkn = f(inputs["gla_out_norm"]), f(inputs["moba_q_norm"]), f(inputs["moba_k_norm"])
    for l in range(DEPTH):
        o = l * NV_L
        vecs[:, o + 0:o + 8] = nm[l].reshape(8, 128).T
        vecs[:, o + 8:o + 16] = nmlp[l].reshape(8, 128).T
        vecs[:, o + 16:o + 24] = nple[l].reshape(8, 128).T
        vecs[:, o + 24:o + 32] = gon[l].reshape(8, 128).T
        vecs[:, o + 32] = mqn[l]
        vecs[:, o + 33] = mkn[l]
    w2aug = np.zeros((DEPTH, 128, 512), np.float32)
    w2aug[:, 0:16, :] = f(inputs["gla_gate_w2"])
    w2aug[:, 16, :] = f(inputs["gla_gate_b"])
    common = {
        "w_in": f(inputs["w_in"]), "w_branch_a": f(inputs["w_branch_a"]), "w_branch_b": f(inputs["w_branch_b"]),
        "w_out": f(inputs["w_out"]), "w_up": f(inputs["w_up"]), "w_down": f(inputs["w_down"]),
        "w_ple_gate": f(inputs["w_ple_gate"]), "w_ple": f(inputs["w_ple"]),
        "w2aug": w2aug, "cf32": cf, "cbf": cb, "laug": laug, "augc": augc, "vecs": vecs,
    }
    x = f(inputs["x"])
    p = f(inputs["p"])
    maps = []
    for c in range(8):
        b = c % 4
        m = dict(common)
        m["xT"] = np.ascontiguousarray(x[b].T)
        m["pT"] = np.ascontiguousarray(p[:, b].transpose(0, 2, 1))
        maps.append(m)
    return maps


def kernel(**inputs):
    if "nc" not in _NC_CACHE:
        _NC_CACHE["nc"] = build()
    nc = _NC_CACHE["nc"]
    maps = _host_layout(inputs)
    res = run_bass_kernel_spmd(nc, maps, core_ids=list(range(8)))
    out = np.stack([np.ascontiguousarray(res.results[b]["outT"].T) for b in range(4)], axis=0)
    return out.astype(np.float32)
```

```python
import contextlib
import numpy as np
import concourse.bass as bass
import concourse.mybir as mybir
from concourse.bass_utils import run_bass_kernel_spmd

F32 = mybir.dt.float32
BF16 = mybir.dt.bfloat16
ALU = mybir.AluOpType
AF = mybir.ActivationFunctionType

ENGS = ("pe", "act", "dve", "pool", "sp")


class Buf:
    __slots__ = ("name", "lw", "rd", "excl", "ver")

    def __init__(self, name, excl=False):
        self.name = name
        self.lw = None
        self.rd = []
        self.ver = 0
        self.excl = excl


class PV:
    __slots__ = ("buf", "ver")

    def __init__(self, buf):
        buf.ver += 1
        self.buf = buf
        self.ver = buf.ver


def _res(bufs):
    out = []
    for b in bufs:
        if isinstance(b, PV):
            assert b.ver == b.buf.ver, "stale PSUM bank handle %s" % b.buf.name
            b = b.buf
        out.append(b)
    return out


class Key:
    __slots__ = ("idx", "cnt", "ldma")

    def __init__(self, idx):
        self.idx = idx
        self.cnt = 0
        self.ldma = None


class Op:
    __slots__ = ("eng", "fn", "deps", "sig", "sigval", "dma", "key", "semval")

    def __init__(self, eng, fn):
        self.eng = eng
        self.fn = fn
        self.deps = []
        self.sig = False
        self.sigval = 0
        self.dma = False
        self.key = None
        self.semval = 0


class Sched:
    def __init__(self, nc):
        self.nc = nc
        self.ops = {e: [] for e in ENGS}
        self.keys = {}

    def key(self, name):
        k = self.keys.get(name)
        if k is None:
            k = Key(len(self.keys))
            self.keys[name] = k
        return k

    def _track(self, op, reads, writes):
        reads = _res(reads)
        writes = _res(writes)
        deps = {}
        for b in reads:
            if b.lw is not None:
                deps[id(b.lw)] = b.lw
            if b.excl:
                for r in b.rd:
                    if r.eng != op.eng:
                        deps[id(r)] = r
        for b in writes:
            if b.lw is not None:
                deps[id(b.lw)] = b.lw
            for r in b.rd:
                deps[id(r)] = r
        for d in deps.values():
            if d is op:
                continue
            if (not d.dma) and d.eng == "pe" and op.eng == "pe" and not op.dma:
                continue
            op.deps.append(d)
            if not d.dma:
                d.sig = True
        for b in reads:
            b.rd.append(op)
        for b in writes:
            b.lw = op
            b.rd = []

    def op(self, eng, fn, reads=(), writes=()):
        o = Op(eng, fn)
        self._track(o, reads, writes)
        self.ops[eng].append(o)
        return o

    def dma(self, q, out_ap, in_ap, reads=(), writes=(), key=None):
        key = self.key(key)
        o = Op(q, lambda e: e.dma_start(out=out_ap, in_=in_ap))
        o.dma = True
        key.cnt += 16
        o.key = key
        o.semval = key.cnt
        self._track(o, reads, writes)
        if key.ldma is not None and all(d is not key.ldma for d in o.deps):
            o.deps.append(key.ldma)
        key.ldma = o
        self.ops[q].append(o)
        return o

    def cc(self, kind, groups, in_ap, out_ap, reads=(), writes=(), key=None):
        key = self.key(key)
        o = Op("pool", lambda e: e.collective_compute(kind, op=ALU.bypass, replica_groups=groups, ins=[in_ap], outs=[out_ap]))
        o.dma = True
        key.cnt += 16
        o.key = key
        o.semval = key.cnt
        self._track(o, reads, writes)
        if key.ldma is not None and all(d is not key.ldma for d in o.deps):
            o.deps.append(key.ldma)
        key.ldma = o
        self.ops["pool"].append(o)
        return o

    def barrier(self):
        lasts = []
        for e in ("pe", "act", "dve", "pool"):
            for o in reversed(self.ops[e]):
                if not o.dma and o.fn is not None:
                    lasts.append(o)
                    break
        dmas = [k.ldma for k in self.keys.values() if k.ldma is not None]
        for e in ENGS:
            o = Op(e, None)
            for d in lasts + dmas:
                if d.eng == "pe" and e == "pe" and not d.dma:
                    continue
                o.deps.append(d)
                if not d.dma:
                    d.sig = True
            self.ops[e].append(o)

    def emit(self):
        nc = self.nc
        with contextlib.ExitStack() as st:
            esem = {e: st.enter_context(nc.semaphore("s_" + e)) for e in ("pe", "act", "dve", "pool")}
            dsem = [st.enter_context(nc.semaphore("d%d" % i)) for i in range(len(self.keys))]
            for e in ("pe", "act", "dve", "pool"):
                c = 0
                for o in self.ops[e]:
                    if o.sig and not o.dma:
                        c += 1
                        o.sigval = c
            block = st.enter_context(nc.Block())

            def run(e, eng):
                waited = {}
                for o in self.ops[e]:
                    for d in o.deps:
                        if d.dma:
                            sem, val = dsem[d.key.idx], d.semval
                        else:
                            sem, val = esem[d.eng], d.sigval
                        k = id(sem)
                        if waited.get(k, 0) < val:
                            eng.wait_ge(sem, val)
                            waited[k] = val
                    if o.fn is None:
                        continue
                    ins = o.fn(eng)
                    if o.dma:
                        ins.then_inc(dsem[o.key.idx], 16)
                    elif o.sig:
                        ins.then_inc(esem[e], 1)

            @block.tensor
            def _(eng):
                run("pe", eng)

            @block.scalar
            def _(eng):
                run("act", eng)

            @block.vector
            def _(eng):
                run("dve", eng)

            @block.gpsimd
            def _(eng):
                run("pool", eng)

            @block.sync
            def _(eng):
                run("sp", eng)


D = 1024
KC = 8
NT = 4096
TT = 512
NTT = 8
DEPTH = 4
PLE = 256
DFF = 4096
O_GQ, O_GK, O_GV, O_GLR, O_GR, O_MQ, O_MK, O_MV, O_GA, O_GB, N_IN = (
    0, 512, 1024, 2048, 2064, 3088, 4112, 5136, 6160, 7184, 8208)
EPS = 1e-6
NV_L = 34
NEG = -30000.0
CF_ID, CF_TRI, CF_TRI2, CF_MBD, CF_GM, CF_AB, NCF = 0, 128, 256, 384, 512, 1024, 1536
CB_ONE, CB_ID, CB_CM, NCB = 0, 128, 256, 512


def host_consts():
    cf = np.zeros((128, NCF), np.float32)
    cf[:, CF_ID:CF_ID + 128] = np.eye(128, dtype=np.float32)
    s = np.arange(128)[:, None]
    t = np.arange(128)[None, :]
    same = (s // 64) == (t // 64)
    cf[:, CF_TRI:CF_TRI + 128] = np.where(same & (s <= t), -1.0 / 16.0, 0.0)
    cf[:, CF_TRI2:CF_TRI2 + 128] = np.where(same & (s > t), -1.0 / 16.0, 0.0)
    cf[:, CF_MBD:CF_MBD + 128] = np.where(same & (s <= t), 1.0, 0.0)
    b = (np.arange(32) // 2)[:, None]
    j = np.arange(16)[None, :]
    gm = np.where(j < b, 0.0, np.where(j == b, 1e30, -1e30)).astype(np.float32)
    cf[:, CF_GM:CF_GM + 512] = gm.reshape(1, 512)
    ab = (-256.0 * (b - j)).astype(np.float32)
    cf[:, CF_AB:CF_AB + 512] = ab.reshape(1, 512)
    cb = np.zeros((128, NCB), np.float32)
    cb[:, CB_ONE:CB_ONE + 128] = 1.0
    cb[:, CB_ID:CB_ID + 128] = np.eye(128, dtype=np.float32)
    kk = np.arange(128)[:, None]
    qq = np.arange(256)[None, :]
    cb[:, CB_CM:CB_CM + 256] = np.where(kk > qq, NEG, 0.0)
    laug = np.zeros((128, 32, 128), np.float32)
    for jj in range(16):
        for c in range(2):
            laug[jj, jj * 2 + c, :] = 1.0
            laug[16, jj * 2 + c, :] = c * 128 + np.arange(128)
            laug[17, jj * 2 + c, :] = 1.0
    laug = laug.reshape(128, 4096)
    slopes = 2.0 ** (-(np.arange(8) + 1.0))
    augc = np.zeros((8, 2, NT), np.float32)
    trel = (np.arange(NT) % 256).astype(np.float32)
    for h in range(8):
        augc[h, 0, :] = slopes[h]
        augc[h, 1, :] = -slopes[h] * trel
    return cf, cb, laug, augc, slopes


def build(depth=DEPTH, debug=False, stop_after=None, dbg=None):
    nc = bass.Bass("TRN2", target_bir_lowering=False)
    slopes = [2.0 ** (-(h + 1.0)) for h in range(8)]

    def din(name, shape, dt=F32):
        return nc.dram_tensor(name, shape, dt, kind="ExternalInput").ap()

    xT_d = din("xT", [D, NT])
    pT_d = din("pT", [DEPTH, PLE, NT])
    w_in_d = din("w_in", [DEPTH, D, N_IN])
    wa_d = din("w_branch_a", [DEPTH, D, D])
    wb_d = din("w_branch_b", [DEPTH, D, D])
    wo_d = din("w_out", [DEPTH, D, D])
    wup_d = din("w_up", [DEPTH, D, DFF])
    wdn_d = din("w_down", [DEPTH, DFF, D])
    wpg_d = din("w_ple_gate", [DEPTH, D, D])
    wple_d = din("w_ple", [DEPTH, PLE, D])
    w2aug_d = din("w2aug", [DEPTH, 128, 512])
    cf_d = din("cf32", [128, NCF])
    cb_d = din("cbf", [128, NCB])
    laug_d = din("laug", [128, 4096])
    augc_d = din("augc", [8, 2, NT])
    vecs_d = din("vecs", [128, DEPTH * NV_L])
    out_d = nc.dram_tensor("outT", [D, NT], F32, kind="ExternalOutput").ap()
    dk = "ExternalOutput" if debug else "Internal"
    xS_d = nc.dram_tensor("xS", [D, NT], F32, kind=dk).ap()
    oaT_d = nc.dram_tensor("oaT", [8, 128, NT], BF16, kind=dk).ap()
    obT_d = nc.dram_tensor("obT", [8, 128, NT], BF16, kind=dk).ap()

    def kcp(ap2d):
        return ap2d.rearrange("(kc p) n -> p kc n", p=128)

    S = Sched(nc)
    with contextlib.ExitStack() as st:
        _nm = [0]

        def sbt(stack, name, shape, dt):
            _nm[0] += 1
            return stack.enter_context(nc.sbuf_tensor("sb%d_%s" % (_nm[0], name), shape, dt))

        hT_t = sbt(st, "hT", [128, NTT, KC, TT], BF16)
        HT = [Buf("hT%d" % i) for i in range(NTT)]

        def hT(tt, kc, lo=0, hi=TT):
            return hT_t[:, tt, kc, lo:hi]

        NSLOT = 6
        slots = []
        for i in range(NSLOT):
            t = sbt(st, "ws%d" % i, [128, 4096], BF16)
            slots.append((t, [Buf("ws%d_%d" % (i, k)) for k in range(4)], i))
        cf_t = sbt(st, "cf", [128, NCF], F32)
        cb_t = sbt(st, "cb", [128, NCB], BF16)
        vec_t = sbt(st, "vec", [128, DEPTH * NV_L], F32)
        gqs_t = sbt(st, "gqs", [128, DEPTH], F32)
        CF, CB, VEC, GQS = Buf("cf"), Buf("cb"), Buf("vec"), Buf("gqs")
        ident_f = cf_t[:, CF_ID:CF_ID + 128]
        triN = cf_t[:, CF_TRI:CF_TRI + 128]
        tri2N = cf_t[:, CF_TRI2:CF_TRI2 + 128]
        maskBD = cf_t[:, CF_MBD:CF_MBD + 128]
        ones_b = cb_t[:, CB_ONE:CB_ONE + 128]
        ident_b = cb_t[:, CB_ID:CB_ID + 128]
        cmask = cb_t[:, CB_CM:CB_CM + 256]
        XB = [Buf("xb0"), Buf("xb1")]
        sq_t = sbt(st, "sq", [128, KC, TT], BF16)
        SQ = Buf("sq")
        rt_t = [sbt(st, "rt%d" % i, [128, TT], F32) for i in range(2)]
        RT = [Buf("rt0"), Buf("rt1")]
        rs_t = [sbt(st, "rs%d" % i, [128, TT], F32) for i in range(2)]
        RS = [Buf("rs0"), Buf("rs1")]
        cnt = {"rt": 0, "big": 0, "half": 0, "any": 0}

        pb = [st.enter_context(nc.psum_tensor("pb%d" % i, [128, 512], F32)) for i in range(8)]
        BIG = [(Buf("big%d" % i, excl=True), pb[i]) for i in range(4)]
        HALF = [(Buf("sm%d" % k, excl=True), pb[4 + k], 0) for k in range(4)]

        ALLB = BIG + [(hb, ht) for (hb, ht, _o) in HALF]

        def big():
            b, t = BIG[cnt["big"] % 4]
            cnt["big"] += 1
            return PV(b), t

        def any8():
            b, t = ALLB[cnt["any"] % 8]
            cnt["any"] += 1
            return PV(b), t

        def half():
            b, t, off = HALF[cnt["half"] % 4]
            cnt["half"] += 1
            return PV(b), (lambda lo, hi, p0=0, p1=128, t=t, off=off: t[p0:p1, off + lo:off + hi])

        def MM(psbuf, items, reads):
            def fn(e, items=items):
                ins = None
                for (o, l, r, s0, s1) in items:
                    ins = e.matmul(o, lhsT=l, rhs=r, start=s0, stop=s1)
                return ins
            S.op("pe", fn, reads=reads, writes=[psbuf])

        def ACTF(out, in_, func, reads, writes, **kw):
            S.op("act", lambda e: e.activation(out=out, in_=in_, func=func, **kw), reads=reads, writes=writes)

        def TT_(out, in0, in1, op, reads, writes, eng="dve"):
            S.op(eng, lambda e: e.tensor_tensor(out=out, in0=in0, in1=in1, op=op), reads=reads, writes=writes)

        def STT(out, in0, scalar, in1, op0, op1, reads, writes, **kw):
            S.op("dve", lambda e: e.scalar_tensor_tensor(out=out, in0=in0, scalar=scalar, in1=in1, op0=op0, op1=op1, **kw),
                 reads=reads, writes=writes)

        def TS(out, in0, s1, s2, op0, op1, reads, writes, eng="dve"):
            if op1 is None:
                S.op(eng, lambda e: e.tensor_scalar(out=out, in0=in0, scalar1=s1, scalar2=None, op0=op0), reads=reads, writes=writes)
            else:
                S.op(eng, lambda e: e.tensor_scalar(out=out, in0=in0, scalar1=s1, scalar2=s2, op0=op0, op1=op1), reads=reads, writes=writes)

        def RSQRT(ps_ap, psbuf, inv_n):
            i = cnt["rt"] % 2
            cnt["rt"] += 1
            ACTF(rt_t[i][:, :], ps_ap, AF.Ln, [psbuf, EPSB], [RT[i]], bias=EPS_AP, scale=inv_n)
            ACTF(rs_t[i][:, :], rt_t[i][:, :], AF.Exp, [RT[i]], [RS[i]], scale=-0.5)
            return RS[i], rs_t[i]

        class WStream:
            def __init__(self):
                self.plan = []
                self.rec = 0
                self.free = [True] * NSLOT
                self.nxt = 0

            def add(self, loads):
                self.plan.append(loads)

            def pump(self):
                while self.rec < len(self.plan) and self.free[self.rec % NSLOT]:
                    t, subs, si = slots[self.rec % NSLOT]
                    for (sidx, outf, in_ap) in self.plan[self.rec]:
                        S.dma("pool", outf(t), in_ap, writes=[subs[k] for k in sidx], key="w%d_%d" % (si, sidx[0]))
                    self.free[self.rec % NSLOT] = False
                    self.rec += 1

            def get(self):
                i = self.nxt
                self.nxt += 1
                self.pump()
                assert self.rec > i, "weight stream stalled"
                t, subs, si = slots[i % NSLOT]
                return i, t, subs

            def release(self, i):
                self.free[i % NSLOT] = True
                self.pump()

        ws = WStream()

        def v3(t, lo, k, n):
            return t[:, lo:lo + k * n].rearrange("p (k n) -> p k n", k=k)

        eps_t = sbt(st, "eps", [128, 1], F32)
        EPSB = Buf("eps")
        EPS_AP = eps_t[:, 0:1]
        S.op("dve", lambda e: e.memset(eps_t[:, :], EPS), writes=[EPSB])
        one_t = sbt(st, "one", [128, 1], F32)
        ONEB = Buf("one")
        ONE_AP = one_t[:, 0:1]
        S.op("dve", lambda e: e.memset(one_t[:, :], 1.0), writes=[ONEB])
        S.dma("sp", cf_t[:, :], cf_d[:, :], writes=[CF], key="cf")
        S.dma("pool", cb_t[:, :], cb_d[:, :], writes=[CB], key="cb")
        S.dma("sp", vec_t[:, :], vecs_d[:, :], writes=[VEC], key="vec")
        for l in range(DEPTH):
            c = l * NV_L + 32
            TS(gqs_t[:, l:l + 1], vec_t[:, c:c + 1], 128.0 ** -0.5, None, ALU.mult, None, [VEC], [GQS])
        _orig_ACTF = ACTF

        def norm_to_hT(xb, x_ap, gcol, tt, big=None):
            big = big or any8
            xbl = xb if isinstance(xb, list) else [xb]
            ACTF(sq_t[:, :, :], x_ap, AF.Square, xbl, [SQ])
            pbuf, pt = big()
            MM(pbuf, [(pt[:, :], ones_b, sq_t[:, kc, :], kc == 0, kc == KC - 1) for kc in range(KC)], [CB, SQ])
            rb, rt = RSQRT(pt[:, :], pbuf, 1.0 / D)

            def fn(e):
                ins = None
                for kc in range(KC):
                    ins = e.scalar_tensor_tensor(out=hT(tt, kc), in0=x_ap[:, kc, :], scalar=vec_t[:, gcol + kc:gcol + kc + 1],
                                                 in1=rt[:, :], op0=ALU.mult, op1=ALU.mult)
                return ins
            S.op("dve", fn, reads=xbl + [VEC, rb], writes=[HT[tt]])

        XS = [Buf("xs%d" % i) for i in range(NTT)]
        OA = {}
        OB = {}

        with contextlib.ExitStack() as ph0:
            xbuf_t = [sbt(ph0, "xb%d" % i, [128, KC, TT], F32) for i in range(2)]
            for tt in range(NTT):
                i = tt % 2
                S.dma("sp", xbuf_t[i][:, :, :], kcp(xT_d)[:, :, tt * TT:(tt + 1) * TT], writes=[XB[i]], key="x%d" % i)
                norm_to_hT(XB[i], xbuf_t[i][:, :, :], 0 * NV_L + 0, tt)


        def plan_G(l):
            wl = kcp(w_in_d[l])
            ws.add([([0, 1, 2, 3], lambda t: v3(t, 0, 8, 512), wl[:, :, O_GQ:O_GQ + 512])])
            ws.add([([0, 1, 2, 3], lambda t: v3(t, 0, 8, 512), wl[:, :, O_GK:O_GK + 512])])
            ws.add([([0, 1, 2, 3], lambda t: v3(t, 0, 8, 512), wl[:, :, O_GV:O_GV + 512])])
            ws.add([([0, 1, 2, 3], lambda t: v3(t, 0, 8, 512), wl[:, :, O_GV + 512:O_GV + 1024])])
            ws.add([([0, 1, 2, 3], lambda t: v3(t, 0, 8, 512), wl[:, :, O_GR:O_GR + 512])])
            ws.add([([0, 1, 2, 3], lambda t: v3(t, 0, 8, 512), wl[:, :, O_GR + 512:O_GR + 1024])])

        def phase_G(l):
            with contextlib.ExitStack() as ph:
                glr_t = sbt(ph, "glrT", [128, TT], BF16)
                GLR = Buf("glr")
                GLRC = Buf("glrc")
                w2_t = sbt(ph, "w2aug", [128, 512], BF16)
                W2 = Buf("w2")
                wglr_t = sbt(ph, "wglr", [128, KC, 16], BF16)
                WGLR = Buf("wglr")
                qs_t = sbt(ph, "qTs", [128, 4, TT], F32)
                ks_t = sbt(ph, "kTs", [128, 4, TT], F32)
                QS = [Buf("qs%d" % h) for h in range(4)]
                KS = [Buf("ks%d" % h) for h in range(4)]
                oacc_t = sbt(ph, "oacc", [128, 8, TT], F32)
                OACC = [Buf("oacc%d" % c) for c in range(8)]
                Sf_t = sbt(ph, "Sf", [128, 4, 2, 256], F32)
                Sb_t = sbt(ph, "Sb", [128, 4, 2, 256], BF16)
                SF = [[Buf("sf%d%d" % (h, i)) for i in range(2)] for h in range(4)]
                SBB = [[Buf("sb%d%d" % (h, i)) for i in range(2)] for h in range(4)]
                e1_t = sbt(ph, "e1", [128, 4, 128], F32)
                l1_t = e1_t
                E1_t = sbt(ph, "E1", [128, 4, 128], F32)
                E2_t = sbt(ph, "E2", [128, 4, 128], F32)
                E3_t = sbt(ph, "E3", [128, 4, 128], F32)
                qd_t = sbt(ph, "qd", [128, 4, 128], BF16)
                kd_t = sbt(ph, "kd", [128, 4, 128], BF16)
                kl0_t = sbt(ph, "kl0", [128, 4, 128], BF16)
                kl1_t = sbt(ph, "kl1", [128, 4, 128], BF16)
                vs_t = sbt(ph, "vs", [128, 4, 256], BF16)
                atm_t = sbt(ph, "atm", [128, 4, 128], BF16)
                Eb = [Buf("e1_%d" % h) for h in range(4)]
                Lb = [Buf("l1_%d" % h) for h in range(4)]
                E1b = [Buf("E1_%d" % h) for h in range(4)]
                E2b = [Buf("E2_%d" % h) for h in range(4)]
                E3b = [Buf("E3_%d" % h) for h in range(4)]
                QD = [Buf("qd%d" % h) for h in range(4)]
                KD = [Buf("kd%d" % h) for h in range(4)]
                KL0 = [Buf("kl0%d" % h) for h in range(4)]
                KL1 = [Buf("kl1%d" % h) for h in range(4)]
                VS = [Buf("vs%d" % h) for h in range(4)]
                ATM = [Buf("atm%d" % h) for h in range(4)]
                sqo_t = sbt(ph, "sqo", [128, 2, TT], BF16)
                SQO = Buf("sqo")
                gs_t = [sbt(ph, "gs%d" % i, [128, TT], BF16) for i in range(4)]
                GS = [Buf("gs%d" % i) for i in range(4)]
                oao_t = [sbt(ph, "oao%d" % i, [128, TT], BF16) for i in range(3)]
                OAO = [Buf("oao%d" % i) for i in range(3)]

                if dbg is not None:
                    dbg.update(dict(glr=glr_t, e1=e1_t, E1=E1_t, E2=E2_t, E3=E3_t, qd=qd_t, kd=kd_t, kl0=kl0_t, kl1=kl1_t,
                                    vs=vs_t, atm=atm_t, Sf=Sf_t, Sb=Sb_t, qs=qs_t, ks=ks_t, oacc=oacc_t, w2=w2_t))
                S.op("dve", lambda e: e.memset(glr_t[:, :], 0.0), writes=[GLRC])
                S.op("dve", lambda e: e.memset(glr_t[0:32, :], 1.0), writes=[GLRC])
                S.op("dve", lambda e: e.memset(kl0_t[:, :, :], 0.0), writes=KL0)
                S.op("dve", lambda e: e.memset(kl1_t[:, :, :], 0.0), writes=KL1)
                S.op("dve", lambda e: e.memset(Sf_t[:, :, :, :], 0.0), writes=[SF[h][0] for h in range(4)])
                S.op("dve", lambda e: e.memset(Sb_t[:, :, :, :], 0.0), writes=[SBB[h][0] for h in range(4)])
                S.dma("pool", w2_t[:, :], w2aug_d[l], writes=[W2], key="w2aug")
                S.dma("pool", wglr_t[:, :, :], kcp(w_in_d[l])[:, :, O_GLR:O_GLR + 16], writes=[WGLR], key="wglr")
                iq, tq, sq_ = ws.get()
                ik, tk, sk_ = ws.get()
                iv0, tv0, sv0 = ws.get()
                iv1, tv1, sv1 = ws.get()
                ir0, tr0, sr0 = ws.get()
                ir1, tr1, sr1 = ws.get()
                wq = v3(tq, 0, 8, 512)
                wk = v3(tk, 0, 8, 512)
                wv = [v3(tv0, 0, 8, 512), v3(tv1, 0, 8, 512)]
                wr = [v3(tr0, 0, 8, 512), v3(tr1, 0, 8, 512)]
                svs = [sv0, sv1]
                srs = [sr0, sr1]

                oa_i = 0
                for tt in range(NTT):
                    pbuf, pt = big()
                    MM(pbuf, [(pt[0:16, :], wglr_t[:, kc, :], hT(tt, kc), kc == 0, kc == KC - 1) for kc in range(KC)],
                       [WGLR, HT[tt]])
                    ACTF(glr_t[0:16, :], pt[0:16, :], AF.Copy, [pbuf, GLRC], [GLR])
                    for h in range(4):
                        pbuf, pt = big()
                        MM(pbuf, [(pt[:, :], wq[:, kc, h * 128:(h + 1) * 128], hT(tt, kc), kc == 0, kc == KC - 1) for kc in range(KC)],
                           sq_ + [HT[tt]])
                        ACTF(qs_t[:, h, :], pt[:, :], AF.Copy, [pbuf], [QS[h]], scale=128.0 ** -0.5)
                        pbuf, pt = big()
                        MM(pbuf, [(pt[:, :], wk[:, kc, h * 128:(h + 1) * 128], hT(tt, kc), kc == 0, kc == KC - 1) for kc in range(KC)],
                           sk_ + [HT[tt]])
                        S.op("dve", lambda e, h=h, pt=pt: e.tensor_copy(out=ks_t[:, h, :], in_=pt[:, :]), reads=[pbuf], writes=[KS[h]])
                    for stt in range(4):
                        t0 = stt * 128
                        for h in range(4):
                            b_, f_ = half()
                            MM(b_, [(f_(0, 128), glr_t[:, t0:t0 + 128], w2_t[:, h * 128:(h + 1) * 128], True, True)], [GLR, GLRC, W2])
                            ACTF(e1_t[:, h, :], f_(0, 128), AF.Exp, [b_], [Eb[h]], scale=-1.0)
                        for h in range(4):
                            b_, f_ = half()
                            MM(b_, [(f_(0, 256), hT(tt, kc, t0, t0 + 128), wv[h // 2][:, kc, (h % 2) * 256:(h % 2) * 256 + 256], kc == 0, kc == KC - 1)
                                    for kc in range(KC)], svs[h // 2] + [HT[tt]])
                            ACTF(vs_t[:, h, :], f_(0, 256), AF.Copy, [b_], [VS[h]])
                        ktp = []
                        for h in range(4):
                            b_, pt = big()
                            MM(b_, [(pt[:, 0:128], hT(tt, kc, t0, t0 + 128), wk[:, kc, h * 128:(h + 1) * 128], kc == 0, kc == KC - 1)
                                    for kc in range(KC)], sk_ + [HT[tt]])
                            ktp.append((b_, pt))
                        for h in range(4):
                            ACTF(l1_t[:, h, :], e1_t[:, h, :], AF.Ln, [Eb[h], ONEB], [Eb[h]], bias=ONE_AP)
                        for h in range(4):
                            b_, f_ = half()
                            MM(b_, [(f_(0, 128), l1_t[:, h, :], triN, True, True)], [Eb[h], CF])
                            ACTF(E1_t[:, h, :], f_(0, 128), AF.Exp, [b_], [E1b[h]])
                            ACTF(E2_t[:, h, :], f_(0, 128), AF.Exp, [b_], [E2b[h]], scale=-1.0)
                            b2, f2 = half()
                            MM(b2, [(f2(0, 128), tri2N, l1_t[:, h, :], True, True)], [Eb[h], CF])
                            ACTF(E3_t[:, h, :], f2(0, 128), AF.Exp, [b2], [E3b[h]])
                        for h in range(4):
                            b_, pt = ktp[h]
                            TT_(kl0_t[0:64, h, :], pt[0:64, 0:128], E3_t[0:64, h, :], ALU.mult, [b_, E3b[h]], [KL0[h]])
                            TT_(kl1_t[64:128, h, :], pt[64:128, 0:128], E3_t[64:128, h, :], ALU.mult, [b_, E3b[h]], [KL1[h]])
                            TT_(qd_t[:, h, :], qs_t[:, h, t0:t0 + 128], E1_t[:, h, :], ALU.mult, [QS[h], E1b[h]], [QD[h]])
                            TT_(kd_t[:, h, :], ks_t[:, h, t0:t0 + 128], E2_t[:, h, :], ALU.mult, [KS[h], E2b[h]], [KD[h]])
                        for h in range(4):
                            b_, f_ = half()
                            MM(b_, [(f_(0, 128), kd_t[:, h, :], qd_t[:, h, :], True, True)], [KD[h], QD[h]])
                            TT_(atm_t[:, h, :], f_(0, 128), maskBD, ALU.mult, [b_, CF], [ATM[h]])
                        for h in range(4):
                            b_, f_ = half()
                            MM(b_, [(f_(0, 256), kl0_t[:, h, :], vs_t[:, h, :], True, True)], [KL0[h], VS[h]])
                            STT(Sf_t[:, h, 1, :], Sf_t[:, h, 0, :], E1_t[:, h, 63:64], f_(0, 256), ALU.mult, ALU.add,
                                [SF[h][0], E1b[h], b_], [SF[h][1]])
                            ACTF(Sb_t[:, h, 1, :], Sf_t[:, h, 1, :], AF.Copy, [SF[h][1]], [SBB[h][1]])
                        for h in range(4):
                            for dvc in range(2):
                                b_, f_ = half()
                                MM(b_, [(f_(0, 128), vs_t[:, h, dvc * 128:(dvc + 1) * 128], atm_t[:, h, :], True, False),
                                        (f_(0, 64), Sb_t[:, h, 0, dvc * 128:(dvc + 1) * 128], qd_t[:, h, 0:64], False, False),
                                        (f_(64, 128), Sb_t[:, h, 1, dvc * 128:(dvc + 1) * 128], qd_t[:, h, 64:128], False, True)],
                                   [VS[h], ATM[h], SBB[h][0], SBB[h][1], QD[h]])
                                c = h * 2 + dvc
                                if dvc == 0:
                                    ACTF(oacc_t[:, c, t0:t0 + 128], f_(0, 128), AF.Copy, [b_], [OACC[c]])
                                else:
                                    S.op("dve", lambda e, c=c, f_=f_, t0=t0: e.tensor_copy(out=oacc_t[:, c, t0:t0 + 128], in_=f_(0, 128)),
                                         reads=[b_], writes=[OACC[c]])
                        for h in range(4):
                            b_, f_ = half()
                            MM(b_, [(f_(0, 256), kl1_t[:, h, :], vs_t[:, h, :], True, True)], [KL1[h], VS[h]])
                            STT(Sf_t[:, h, 0, :], Sf_t[:, h, 1, :], E1_t[:, h, 127:128], f_(0, 256), ALU.mult, ALU.add,
                                [SF[h][1], E1b[h], b_], [SF[h][0]])
                            ACTF(Sb_t[:, h, 0, :], Sf_t[:, h, 0, :], AF.Copy, [SF[h][0]], [SBB[h][0]])
                    for hp in range(2):
                        rr = {}
                        for h in (2 * hp, 2 * hp + 1):
                            ACTF(sqo_t[:, :, :], oacc_t[:, 2 * h:2 * h + 2, :], AF.Square, [OACC[2 * h], OACC[2 * h + 1]], [SQO])
                            pbuf, pt = big()
                            MM(pbuf, [(pt[:, :], ones_b, sqo_t[:, 0, :], True, False), (pt[:, :], ones_b, sqo_t[:, 1, :], False, True)], [CB, SQO])
                            rr[h] = RSQRT(pt[:, :], pbuf, 1.0 / 256.0)
                        for h in (2 * hp, 2 * hp + 1):
                            rb, rt = rr[h]
                            for dvc in range(2):
                                c = h * 2 + dvc
                                pbuf, pt = big()
                                MM(pbuf, [(pt[:, :], wr[c // 4][:, kc, (c % 4) * 128:(c % 4) * 128 + 128], hT(tt, kc), kc == 0, kc == KC - 1)
                                          for kc in range(KC)], srs[c // 4] + [HT[tt]])
                                gi = c % 4
                                ACTF(gs_t[gi][:, :], pt[:, :], AF.Silu, [pbuf], [GS[gi]])
                                gcol = l * NV_L + 24 + c
                                STT(oacc_t[:, c, :], oacc_t[:, c, :], vec_t[:, gcol:gcol + 1], rt[:, :], ALU.mult, ALU.mult,
                                    [OACC[c], VEC, rb], [OACC[c]])
                                oi = oa_i % 3
                                oa_i += 1
                                TT_(oao_t[oi][:, :], oacc_t[:, c, :], gs_t[gi][:, :], ALU.mult, [OACC[c], GS[gi]], [OAO[oi]])
                                OA[(c, tt)] = Buf("oa%d_%d" % (c, tt))
                                S.dma("sp", oaT_d[c, :, tt * TT:(tt + 1) * TT], oao_t[oi][:, :], reads=[OAO[oi]], writes=[OA[(c, tt)]],
                                      key="oast%d" % oi)
                for i in (iq, ik, iv0, iv1, ir0, ir1):
                    ws.release(i)
            S.barrier()

        def plan_M(l):
            wl = kcp(w_in_d[l])
            for h in range(8):
                ws.add([([0], lambda t: v3(t, 0, 8, 128), wl[:, :, O_MQ + h * 128:O_MQ + (h + 1) * 128]),
                        ([1], lambda t: v3(t, 1024, 8, 128), wl[:, :, O_MK + h * 128:O_MK + (h + 1) * 128]),
                        ([2], lambda t: v3(t, 2048, 8, 128), wl[:, :, O_MV + h * 128:O_MV + (h + 1) * 128])])

        def phase_M(l):
            with contextlib.ExitStack() as ph:
                laug_t = sbt(ph, "laug", [128, 32, 128], BF16)
                LAUG = Buf("laug")
                QT_t = sbt(ph, "QT", [128, NT], BF16)
                KT_t = sbt(ph, "KT", [128, NT], BF16)
                V_t = sbt(ph, "Vp", [128, 32, 129], BF16)
                raug_t = sbt(ph, "raug", [128, NT], BF16)
                obt_t = sbt(ph, "obt", [128, NT], BF16)
                QTB = [Buf("qt%d" % i) for i in range(NTT)]
                KTB = [Buf("kt%d" % i) for i in range(NTT)]
                VB = [Buf("v%d" % i) for i in range(32)]
                VONE = Buf("vone")
                RA = [Buf("ra%d" % i) for i in range(16)]
                RAZ = Buf("raz")
                RAC = Buf("rac")
                OBB = [Buf("obb%d" % i) for i in range(16)]
                ksum_t = sbt(ph, "ksum", [128, 16], F32)
                KSUM = [Buf("ksum%d" % i) for i in range(16)]
                kmT_t = sbt(ph, "kmT", [128, 16], BF16)
                KMT = Buf("kmt")
                qf_t = [sbt(ph, "qf%d" % i, [128, TT], F32) for i in range(2)]
                QF = [Buf("qf0"), Buf("qf1")]
                sqq_t = [sbt(ph, "sqq%d" % i, [128, TT], BF16) for i in range(2)]
                SQQ = [Buf("sqq0"), Buf("sqq1")]
                g1_t = sbt(ph, "g1", [128, 512], F32)
                m8_t = sbt(ph, "m8", [128, 256], F32)
                tg_t = sbt(ph, "tg", [128, 512], F32)
                mb_t = sbt(ph, "mb", [128, 512], F32)
                G1, M8, TG, MB = Buf("g1"), Buf("m8"), Buf("tg"), Buf("mb")
                NP = 4
                pT_t = [sbt(ph, "pT%d" % i, [128, 512], BF16) for i in range(NP)]
                PT = [Buf("pT%d" % i) for i in range(NP)]
                rd_t = [sbt(ph, "rd%d" % i, [128, 1], F32) for i in range(4)]
                RD = [Buf("rd%d" % i) for i in range(4)]
                on_t = [sbt(ph, "on%d" % i, [128, 128], F32) for i in range(4)]
                ON = [Buf("on%d" % i) for i in range(4)]

                S.dma("pool", laug_t[:, :, :], laug_d.rearrange("p (v k) -> p v k", v=32), writes=[LAUG], key="laug")
                S.op("dve", lambda e: e.memset(V_t[:, :, :], 1.0), writes=[VONE])
                S.op("dve", lambda e: e.memset(raug_t[:, :], 0.0), writes=[RAZ])
                pcnt = 0
                ocnt = 0
                gcnt = 0
                for h in range(8):
                    iw, tw, sw = ws.get()
                    wmq = v3(tw, 0, 8, 128)
                    wmk = v3(tw, 1024, 8, 128)
                    wmv = v3(tw, 2048, 8, 128)
                    S.dma("pool", raug_t[16:18, :], augc_d[h], reads=[RAZ], writes=[RAC], key="raugc")
                    gq_ap = gqs_t[:, l:l + 1]
                    gk_ap = vec_t[:, l * NV_L + 33:l * NV_L + 34]
                    for tt in range(NTT):
                        pq, ptq = big()
                        MM(pq, [(ptq[:, :], wmq[:, kc, :], hT(tt, kc), kc == 0, kc == KC - 1) for kc in range(KC)], [sw[0], HT[tt]])
                        pk, ptk = big()
                        MM(pk, [(ptk[:, :], wmk[:, kc, :], hT(tt, kc), kc == 0, kc == KC - 1) for kc in range(KC)], [sw[1], HT[tt]])
                        ACTF(sqq_t[0][:, :], ptq[:, :], AF.Square, [pq], [SQQ[0]])
                        ACTF(qf_t[0][:, :], ptq[:, :], AF.Copy, [pq], [QF[0]])
                        ACTF(sqq_t[1][:, :], ptk[:, :], AF.Square, [pk], [SQQ[1]])
                        ACTF(qf_t[1][:, :], ptk[:, :], AF.Copy, [pk], [QF[1]])
                        pb2, pt2 = big()
                        MM(pb2, [(pt2[:, :], ones_b, sqq_t[0][:, :], True, True)], [CB, SQQ[0]])
                        rb, rt = RSQRT(pt2[:, :], pb2, 1.0 / 128.0)
                        STT(QT_t[:, tt * TT:(tt + 1) * TT], qf_t[0][:, :], gq_ap, rt[:, :], ALU.mult, ALU.mult,
                            [QF[0], GQS, rb], [QTB[tt]])
                        for stt in range(4):
                            ti = tt * 4 + stt
                            b_, f_ = half()
                            MM(b_, [(f_(0, 128), hT(tt, kc, stt * 128, stt * 128 + 128), wmv[:, kc, :], kc == 0, kc == KC - 1)
                                    for kc in range(KC)], [sw[2], HT[tt]])
                            S.op("dve", lambda e, f_=f_, ti=ti: e.tensor_copy(out=V_t[:, ti, 0:128], in_=f_(0, 128)), reads=[b_, VONE], writes=[VB[ti]])
                        pb3, pt3 = big()
                        MM(pb3, [(pt3[:, :], ones_b, sqq_t[1][:, :], True, True)], [CB, SQQ[1]])
                        rb, rt = RSQRT(pt3[:, :], pb3, 1.0 / 128.0)
                        for blk in range(2):
                            bi = tt * 2 + blk
                            lo = blk * 256
                            STT(KT_t[:, tt * TT + lo:tt * TT + lo + 256], qf_t[1][:, lo:lo + 256], gk_ap, rt[:, lo:lo + 256],
                                ALU.mult, ALU.mult, [QF[1], VEC, rb], [KTB[tt], KSUM[bi]], accum_out=ksum_t[:, bi:bi + 1])
                    ws.release(iw)
                    TS(kmT_t[:, :], ksum_t[:, :], 1.0 / 256.0, None, ALU.mult, None, KSUM, [KMT])
                    gb_, gt_ = big()
                    MM(gb_, [(gt_[:, qt * 16:(qt + 1) * 16], QT_t[:, qt * 128:(qt + 1) * 128], kmT_t[:, :], True, True) for qt in range(32)],
                       QTB + [KMT])
                    TT_(g1_t[:, :], gt_[:, :], cf_t[:, CF_GM:CF_GM + 512], ALU.add, [gb_, CF], [G1])

                    def fmax(e):
                        ins = None
                        for qt in range(32):
                            ins = e.max(out=m8_t[:, qt * 8:(qt + 1) * 8], in_=g1_t[:, qt * 16:(qt + 1) * 16])
                        return ins
                    S.op("dve", fmax, reads=[G1], writes=[M8])

                    def fthr(e):
                        ins = None
                        for qt in range(32):
                            ins = e.tensor_scalar(out=tg_t[:, qt * 16:(qt + 1) * 16], in0=g1_t[:, qt * 16:(qt + 1) * 16],
                                                  scalar1=m8_t[:, qt * 8 + 3:qt * 8 + 4], scalar2=NEG, op0=ALU.is_lt, op1=ALU.mult)
                        return ins
                    S.op("dve", fthr, reads=[G1, M8], writes=[TG])
                    STT(mb_t[:, :], cf_t[:, CF_AB:CF_AB + 512], slopes[h], tg_t[:, :], ALU.mult, ALU.add, [CF, TG], [MB])
                    for g in range(8):
                        b2, t2 = big()

                        def ftr(e, g=g, t2=t2):
                            ins = None
                            for k in range(4):
                                qt = g * 4 + k
                                ins = e.transpose(out=t2[0:16, k * 128:(k + 1) * 128], in_=mb_t[:, qt * 16:(qt + 1) * 16], identity=ident_f)
                            return ins
                        S.op("pe", ftr, reads=[MB, CF], writes=[b2])
                        ACTF(raug_t[0:16, g * 512:(g + 1) * 512], t2[0:16, :], AF.Copy, [b2, RAZ], [RA[2 * g], RA[2 * g + 1]])
                    pending = []

                    def flush_epilogue():
                        for (ons, q0_, b_i) in pending:
                            for hf, oi in enumerate(ons):
                                b2, t2 = big()
                                S.op("pe", lambda e, t2=t2, oi=oi: e.transpose(out=t2[:, 0:128], in_=on_t[oi][:, :], identity=ident_f),
                                     reads=[ON[oi], CF], writes=[b2])
                                S.op("dve", lambda e, t2=t2, q0_=q0_, hf=hf: e.tensor_copy(out=obt_t[:, q0_ + hf * 128:q0_ + hf * 128 + 128], in_=t2[:, 0:128]),
                                     reads=[b2], writes=[OBB[b_i]])
                        del pending[:]

                    for b in range(16):
                        q0 = b * 256
                        oAb, oAt = big()
                        oBb, oBt = big()
                        units = list(range(b + 1))
                        n = len(units)
                        sps = [None] * n
                        firstA = [True]
                        firstB = [True]

                        def issue_S(i):
                            j = units[i]
                            b_, f_ = half()
                            k0 = j * 256
                            rds = [KTB[k0 // TT], QTB[q0 // TT], LAUG, RA[b], RAC, RAZ, CB]
                            if j < b:
                                items = []
                                for c in range(2):
                                    items += [(f_(c * 256, c * 256 + 256), KT_t[:, k0 + c * 128:k0 + c * 128 + 128], QT_t[:, q0:q0 + 256], True, False),
                                              (f_(c * 256, c * 256 + 256), laug_t[:, j * 2 + c, :], raug_t[:, q0:q0 + 256], False, True)]
                            else:
                                items = [(f_(0, 256), KT_t[:, k0:k0 + 128], QT_t[:, q0:q0 + 256], True, False),
                                         (f_(0, 256), laug_t[:, j * 2, :], raug_t[:, q0:q0 + 256], False, False),
                                         (f_(0, 256), ident_b, cmask, False, True),
                                         (f_(256, 384), KT_t[:, k0 + 128:k0 + 256], QT_t[:, q0 + 128:q0 + 256], True, False),
                                         (f_(256, 384), laug_t[:, j * 2 + 1, :], raug_t[:, q0 + 128:q0 + 256], False, False),
                                         (f_(256, 384), ident_b, cmask[:, 0:128], False, True)]
                            MM(b_, items, rds)
                            sps[i] = (b_, f_)

                        issue_S(0)
                        if n > 1:
                            issue_S(1)
                        for i in range(n):
                            if i + 2 < n:
                                issue_S(i + 2)
                            j = units[i]
                            b_, f_ = sps[i]
                            pi = pcnt % NP
                            pcnt += 1
                            own = (j == b)
                            w = 384 if own else 512
                            ACTF(pT_t[pi][:, 0:w], f_(0, w), AF.Exp, [b_], [PT[pi]])
                            rdsA = [PT[pi], VB[j * 2], VONE]
                            rdsB = [PT[pi], VB[j * 2], VB[j * 2 + 1], VONE]
                            if not own:
                                MM(oAb, [(oAt[:, 0:129], pT_t[pi][:, 0:128], V_t[:, j * 2, :], firstA[0], False),
                                         (oAt[:, 0:129], pT_t[pi][:, 256:384], V_t[:, j * 2 + 1, :], False, False)], rdsB)
                                MM(oBb, [(oBt[:, 0:129], pT_t[pi][:, 128:256], V_t[:, j * 2, :], firstB[0], False),
                                         (oBt[:, 0:129], pT_t[pi][:, 384:512], V_t[:, j * 2 + 1, :], False, False)], rdsB)
                            else:
                                MM(oAb, [(oAt[:, 0:129], pT_t[pi][:, 0:128], V_t[:, j * 2, :], firstA[0], True)], rdsA)
                                MM(oBb, [(oBt[:, 0:129], pT_t[pi][:, 128:256], V_t[:, j * 2, :], firstB[0], False),
                                         (oBt[:, 0:129], pT_t[pi][:, 256:384], V_t[:, j * 2 + 1, :], False, True)], rdsB)
                            firstA[0] = False
                            firstB[0] = False
                            if i == 0:
                                flush_epilogue()
                        ons = []
                        for hf, (ob_, ot_) in enumerate(((oAb, oAt), (oBb, oBt))):
                            oi = ocnt % 4
                            ocnt += 1
                            S.op("dve", lambda e, oi=oi, ot_=ot_: e.reciprocal(out=rd_t[oi][:, :], in_=ot_[:, 128:129]), reads=[ob_], writes=[RD[oi]])
                            TS(on_t[oi][:, :], ot_[:, 0:128], rd_t[oi][:, 0:1], None, ALU.mult, None, [ob_, RD[oi]], [ON[oi]])
                            ons.append(oi)
                        pending.append((ons, q0, b))
                    flush_epilogue()
                    OB[h] = Buf("ob%d" % h)
                    S.dma("sp", obT_d[h, :, :], obt_t[:, :], reads=OBB, writes=[OB[h]], key="obst")
            S.barrier()

        def plan_T(l):
            wl = kcp(w_in_d[l])
            for tt in range(NTT):
                for oc in range(8):
                    cs = slice(oc * 128, (oc + 1) * 128)
                    ws.add([([0], lambda t: v3(t, 0, 8, 128), kcp(wa_d[l])[:, :, cs]),
                            ([1], lambda t: v3(t, 1024, 8, 128), wl[:, :, O_GA + oc * 128:O_GA + (oc + 1) * 128]),
                            ([2], lambda t: v3(t, 2048, 8, 128), kcp(wb_d[l])[:, :, cs]),
                            ([3], lambda t: v3(t, 3072, 8, 128), wl[:, :, O_GB + oc * 128:O_GB + (oc + 1) * 128])])
                for og in range(2):
                    ws.add([([k], (lambda t, k=k: v3(t, k * 1024, 8, 128)), kcp(wo_d[l])[:, :, (og * 4 + k) * 128:(og * 4 + k + 1) * 128])
                            for k in range(4)])
                for fh in range(2):
                    for fg in range(4):
                        c0 = fh * 2048 + fg * 512
                        ws.add([([0, 1, 2, 3], lambda t: v3(t, 0, 8, 512), kcp(wup_d[l])[:, :, c0:c0 + 512])])
                    for oc in range(8):
                        ws.add([([0, 1], lambda t: v3(t, 0, 16, 128),
                                 kcp(wdn_d[l][fh * 2048:(fh + 1) * 2048, :])[:, :, oc * 128:(oc + 1) * 128])])
                for og in range(4):
                    lo = []
                    for k in range(2):
                        oc = og * 2 + k
                        lo.append(([2 * k], (lambda t, k=k: v3(t, 2 * k * 1024, 8, 128)), kcp(wpg_d[l])[:, :, oc * 128:(oc + 1) * 128]))
                        lo.append(([2 * k + 1], (lambda t, k=k: v3(t, (2 * k + 1) * 1024, 2, 128)), kcp(wple_d[l])[:, :, oc * 128:(oc + 1) * 128]))
                    ws.add(lo)

        def phase_T(l, last):
            with contextlib.ExitStack() as ph:
                xbuf_t = [sbt(ph, "xb%d" % i, [128, KC, TT], F32) for i in range(2)]
                aT_t = sbt(ph, "aT", [128, 24, TT], BF16)
                ATC = [Buf("atc%d" % i) for i in range(24)]
                AT = [ATC[0:8], ATC[8:16], ATC[16:24]]
                XC = [[Buf("xc%d_%d" % (i, c)) for c in range(KC)] for i in range(2)]
                ptb_t = sbt(ph, "ptb", [128, 2, TT], BF16)
                PTB = Buf("ptb")
                sa_t = [sbt(ph, "sa%d" % i, [128, TT], F32) for i in range(2)]
                SA = [Buf("sa0"), Buf("sa1")]
                sbg_t = [sbt(ph, "sbg%d" % i, [128, TT], F32) for i in range(2)]
                SBG = [Buf("sbg0"), Buf("sbg1")]
                rl_t = [sbt(ph, "rl%d" % i, [128, TT], F32) for i in range(2)]
                RL = [Buf("rl0"), Buf("rl1")]
                src_d = xT_d if l == 0 else xS_d
                dst_d = out_d if last else xS_d
                def issue_loads(t_):
                    xi_ = t_ % 2
                    tsl_ = slice(t_ * TT, (t_ + 1) * TT)
                    S.dma("sp", xbuf_t[xi_][:, :, :], kcp(src_d)[:, :, tsl_], reads=([XS[t_]] if l > 0 else []), writes=XC[xi_], key="x%d" % xi_)
                    S.dma("sp", aT_t[:, 0:8, :], oaT_d.rearrange("c p t -> p c t")[:, :, tsl_], reads=[OA[(c, t_)] for c in range(8)],
                          writes=AT[0], key="ata")
                    S.dma("sp", aT_t[:, 8:16, :], obT_d.rearrange("c p t -> p c t")[:, :, tsl_], reads=[OB[h] for h in range(8)],
                          writes=AT[1], key="atb")

                issue_loads(0)
                deferred = []
                for tt in range(NTT):
                    xi = tt % 2
                    xb, xt = XC[xi], xbuf_t[xi]
                    tsl = slice(tt * TT, (tt + 1) * TT)
                    S.dma("pool", ptb_t[:, :, :], kcp(pT_d[l])[:, :, tsl], writes=[PTB], key="ptb")
                    for oc in range(8):
                        iw, tw, sw = ws.get()
                        w4 = [v3(tw, k * 1024, 8, 128) for k in range(4)]
                        i2 = oc % 2
                        pa, pat = any8()
                        MM(pa, [(pat[:, :], w4[0][:, kc, :], aT_t[:, kc, :], kc == 0, kc == KC - 1) for kc in range(KC)], [sw[0]] + AT[0])
                        pg, pgt = any8()
                        MM(pg, [(pgt[:, :], w4[1][:, kc, :], hT(tt, kc), kc == 0, kc == KC - 1) for kc in range(KC)], [sw[1], HT[tt]])
                        ACTF(sa_t[i2][:, :], pgt[:, :], AF.Sigmoid, [pg], [SA[i2]])
                        TT_(sa_t[i2][:, :], pat[:, :], sa_t[i2][:, :], ALU.mult, [pa, SA[i2]], [SA[i2]])
                        pb_, pbt = any8()
                        MM(pb_, [(pbt[:, :], w4[2][:, kc, :], aT_t[:, 8 + kc, :], kc == 0, kc == KC - 1) for kc in range(KC)], [sw[2]] + AT[1])
                        pg2, pg2t = any8()
                        MM(pg2, [(pg2t[:, :], w4[3][:, kc, :], hT(tt, kc), kc == 0, kc == KC - 1) for kc in range(KC)], [sw[3], HT[tt]])
                        ws.release(iw)
                        ACTF(sbg_t[i2][:, :], pg2t[:, :], AF.Sigmoid, [pg2], [SBG[i2]])
                        TT_(sbg_t[i2][:, :], pbt[:, :], sbg_t[i2][:, :], ALU.mult, [pb_, SBG[i2]], [SBG[i2]])
                        TT_(aT_t[:, 16 + oc, :], sa_t[i2][:, :], sbg_t[i2][:, :], ALU.add, [SA[i2], SBG[i2]], [ATC[16 + oc]])
                    while deferred:
                        dxb, dxt, dtt = deferred.pop(0)
                        norm_to_hT(dxb, dxt[:, :, :], (l + 1) * NV_L + 0, dtt)
                    for og in range(2):
                        iw, tw, sw = ws.get()
                        for k in range(4):
                            oc = og * 4 + k
                            wv_ = v3(tw, k * 1024, 8, 128)
                            pd, pdt = any8()
                            MM(pd, [(pdt[:, :], wv_[:, kc, :], aT_t[:, 16 + kc, :], kc == 0, kc == KC - 1) for kc in range(KC)], [sw[k]] + AT[2])
                            TT_(xt[:, oc, :], xt[:, oc, :], pdt[:, :], ALU.add, [xb[oc], pd], [xb[oc]])
                        ws.release(iw)
                    norm_to_hT(xb, xt[:, :, :], l * NV_L + 8, tt)
                    for fh in range(2):
                        for fg in range(4):
                            iw, tw, sw = ws.get()
                            wu = v3(tw, 0, 8, 512)
                            for k in range(4):
                                ffc = fg * 4 + k
                                i2 = ffc % 2
                                pu, put = any8()
                                MM(pu, [(put[:, :], wu[:, kc, k * 128:(k + 1) * 128], hT(tt, kc), kc == 0, kc == KC - 1) for kc in range(KC)],
                                   sw + [HT[tt]])
                                ACTF(rl_t[i2][:, :], put[:, :], AF.Relu, [pu], [RL[i2]])
                                TT_(aT_t[:, ffc, :], rl_t[i2][:, :], rl_t[i2][:, :], ALU.mult, [RL[i2]], [ATC[ffc]])
                            ws.release(iw)
                        for oc in range(8):
                            iw, tw, sw = ws.get()
                            wd = v3(tw, 0, 16, 128)
                            pd, pdt = any8()
                            MM(pd, [(pdt[:, :], wd[:, kc, :], aT_t[:, kc, :], kc == 0, kc == 15) for kc in range(16)], sw[0:2] + ATC[0:16])
                            ws.release(iw)
                            TT_(xt[:, oc, :], xt[:, oc, :], pdt[:, :], ALU.add, [xb[oc], pd], [xb[oc]])
                    if tt + 1 < NTT:
                        issue_loads(tt + 1)
                    norm_to_hT(xb, xt[:, :, :], l * NV_L + 16, tt)
                    for og in range(4):
                        iw, tw, sw = ws.get()
                        for k in range(2):
                            oc = og * 2 + k
                            i2 = oc % 2
                            wg = v3(tw, 2 * k * 1024, 8, 128)
                            wp = v3(tw, (2 * k + 1) * 1024, 2, 128)
                            pg, pgt = any8()
                            MM(pg, [(pgt[:, :], wg[:, kc, :], hT(tt, kc), kc == 0, kc == KC - 1) for kc in range(KC)], [sw[2 * k], HT[tt]])
                            pe_, pet = any8()
                            MM(pe_, [(pet[:, :], wp[:, kc, :], ptb_t[:, kc, :], kc == 0, kc == 1) for kc in range(2)], [sw[2 * k + 1], PTB])
                            ACTF(sa_t[i2][:, :], pgt[:, :], AF.Sigmoid, [pg], [SA[i2]])
                            TT_(sa_t[i2][:, :], pet[:, :], sa_t[i2][:, :], ALU.mult, [pe_, SA[i2]], [SA[i2]])
                            TT_(xt[:, oc, :], xt[:, oc, :], sa_t[i2][:, :], ALU.add, [xb[oc], SA[i2]], [xb[oc]])
                        ws.release(iw)
                    S.dma("sp", kcp(dst_d)[:, :, tsl], xt[:, :, :], reads=xb, writes=[XS[tt]], key="xst%d" % xi)
                    if not last:
                        if tt == NTT - 1:
                            norm_to_hT(xb, xt[:, :, :], (l + 1) * NV_L + 0, tt)
                        else:
                            deferred.append((xb, xt, tt))
            S.barrier()

        S.barrier()

        plan_G(0)
        for l in range(depth):
            last = (l == depth - 1)
            plan_M(l)
            phase_G(l)
            if stop_after == "G":
                break
            plan_T(l)
            phase_M(l)
            if stop_after == "M":
                break
            if not last:
                plan_G(l + 1)
            phase_T(l, last)
        S.op("sp", lambda e: e.nop(), reads=XS)
        S.emit()
    return nc


_NC_CACHE = {}


def _host_layout(inputs):
    cf, cb, laug, augc, _ = host_consts()
    vecs = np.zeros((128, DEPTH * NV_L), np.float32)
    f = lambda a: np.asarray(a, dtype=np.float32)
    nm, nmlp, nple = f(inputs["norm_mix"]), f(inputs["norm_mlp"]), f(inputs["norm_ple"])
    gon, mqn, mkn = f(inputs["gla_out_norm"]), f(inputs["moba_q_norm"]), f(inputs["moba_k_norm"])
    for l in range(DEPTH):
        o = l * NV_L
        vecs[:, o + 0:o + 8] = nm[l].reshape(8, 128).T
        vecs[:, o + 8:o + 16] = nmlp[l].reshape(8, 128).T
        vecs[:, o + 16:o + 24] = nple[l].reshape(8, 128).T
        vecs[:, o + 24:o + 32] = gon[l].reshape(8, 128).T
        vecs[:, o + 32] = mqn[l]
        vecs[:, o + 33] = mkn[l]
    w2aug = np.zeros((DEPTH, 128, 512), np.float32)
    w2aug[:, 0:16, :] = f(inputs["gla_gate_w2"])
    w2aug[:, 16, :] = f(inputs["gla_gate_b"])
    common = {
        "w_in": f(inputs["w_in"]), "w_branch_a": f(inputs["w_branch_a"]), "w_branch_b": f(inputs["w_branch_b"]),
        "w_out": f(inputs["w_out"]), "w_up": f(inputs["w_up"]), "w_down": f(inputs["w_down"]),
        "w_ple_gate": f(inputs["w_ple_gate"]), "w_ple": f(inputs["w_ple"]),
        "w2aug": w2aug, "cf32": cf, "cbf": cb, "laug": laug, "augc": augc, "vecs": vecs,
    }
    x = f(inputs["x"])
    p = f(inputs["p"])
    maps = []
    for c in range(8):
        b = c % 4
        m = dict(common)
        m["xT"] = np.ascontiguousarray(x[b].T)
        m["pT"] = np.ascontiguousarray(p[:, b].transpose(0, 2, 1))
        maps.append(m)
    return maps


def kernel(**inputs):
    if "nc" not in _NC_CACHE:
        _NC_CACHE["nc"] = build()
    nc = _NC_CACHE["nc"]
    maps = _host_layout(inputs)
    res = run_bass_kernel_spmd(nc, maps, core_ids=list(range(8)))
    out = np.stack([np.ascontiguousarray(res.results[b]["outT"].T) for b in range(4)], axis=0)
    return out.astype(np.float32)
```
